# Optimizing a Trainium2 kernel written in Bass

```python
import math
import jax, jax.numpy as jnp
from jax import lax
import numpy as np

D_MODEL = 1024
BATCH = 8
SEQ = 4096
DEPTH = 4

HEAD_DIM = 64
H_DIL = 6
H_MLA = 6
H_MOBA = 4
D_DIL = H_DIL * HEAD_DIM
MLA_NOPE = 64
MLA_ROPE = 32
MLA_V = 64
MLA_Q_LORA = 384
MLA_KV_LORA = 128
D_MLA = H_MLA * MLA_V
D_MOBA = H_MOBA * HEAD_DIM
D_MIX = D_DIL + D_MLA + D_MOBA
DILATED_PATTERNS = ((128, 1), (512, 4), (2048, 16))
MOBA_BLOCK = 256
MOBA_TOPK = 3
MOBA_QBLK = 64
DENSE_QBLK = 128
NUM_BUCKETS = 32
MAX_EXACT = 16
REL_MAX_DISTANCE = 2048
ROPE_THETA = 10000.0
D_FF = 4 * D_MODEL
EPS = 1e-6
SPLIT_SIZES = (D_DIL, D_DIL, D_DIL, MLA_Q_LORA, MLA_KV_LORA, MLA_ROPE, D_MOBA, D_MOBA, D_MOBA)
D_IN = D_DIL * 3 + MLA_Q_LORA + MLA_KV_LORA + MLA_ROPE + D_MOBA * 3

kernel_name = 'hybrid_dilated_mla_moba_block'


def rmsnorm(x, g):
    xf = x.astype(jnp.float32)
    y = xf * lax.rsqrt(jnp.mean(xf * xf, axis=-1, keepdims=True) + EPS)
    return (y * g.astype(jnp.float32)).astype(x.dtype)


def t5_bucket(dist):
    n = jnp.maximum(dist, 0)
    nf = jnp.maximum(n, 1).astype(jnp.float32)
    large = MAX_EXACT + (jnp.log(nf / MAX_EXACT) / math.log(REL_MAX_DISTANCE / MAX_EXACT)
                         * (NUM_BUCKETS - MAX_EXACT)).astype(jnp.int32)
    large = jnp.minimum(large, NUM_BUCKETS - 1)
    return jnp.where(n < MAX_EXACT, n, large)


def rope(x, cos, sin):
    half = x.shape[-1] // 2
    xf = x.astype(jnp.float32)
    x1, x2 = xf[..., :half], xf[..., half:]
    return jnp.concatenate([x1 * cos - x2 * sin, x2 * cos + x1 * sin], axis=-1).astype(x.dtype)


def banded_attention(q, k, v, bias, span):
    N, H, L, dh = q.shape
    nb = -(-L // span)
    pad = nb * span - L
    padw = ((0, 0), (0, 0), (0, pad), (0, 0))
    qb = jnp.pad(q, padw).reshape(N, H, nb, span, dh)
    kb = jnp.pad(k, padw).reshape(N, H, nb, span, dh)
    vb = jnp.pad(v, padw).reshape(N, H, nb, span, dh)
    shift = ((0, 0), (0, 0), (1, 0), (0, 0), (0, 0))
    kk = jnp.concatenate([jnp.pad(kb, shift)[:, :, :-1], kb], axis=3)
    vv = jnp.concatenate([jnp.pad(vb, shift)[:, :, :-1], vb], axis=3)
    s = jnp.einsum('nhiqd,nhikd->nhiqk', qb, kk).astype(jnp.float32) * (dh ** -0.5)
    s = s + bias[None, :, None].astype(jnp.float32)
    a = jnp.arange(span)[:, None]
    j = jnp.arange(2 * span)[None, :]
    diff = span + a - j
    in_band = (diff >= 0) & (diff <= span)
    has_prev = (jnp.arange(nb) > 0)[:, None, None] | (j >= span)[None]
    mask = in_band[None] & has_prev
    s = jnp.where(mask, s, -jnp.inf)
    lse = jax.nn.logsumexp(s, axis=-1)
    p = jnp.exp(s - lse[..., None]).astype(v.dtype)
    o = jnp.einsum('nhiqk,nhikd->nhiqd', p, vv).reshape(N, H, nb * span, dh)[:, :, :L]
    return o, lse.reshape(N, H, nb * span)[:, :, :L]


def dilated_mixture(q, k, v, bias_tab):
    B, S, H, dh = q.shape
    outs, lses = [], []
    for window, dil in DILATED_PATTERNS:
        L = S // dil
        span = window // dil

        def to_sub(t):
            return t.reshape(B, L, dil, H, dh).transpose(0, 2, 3, 1, 4).reshape(B * dil, H, L, dh)

        diff = span + jnp.arange(span)[:, None] - jnp.arange(2 * span)[None, :]
        bias = bias_tab[t5_bucket(diff * dil)].transpose(2, 0, 1)
        o, lse = banded_attention(to_sub(q), to_sub(k), to_sub(v), bias, span)
        outs.append(o.reshape(B, dil, H, L, dh).transpose(0, 3, 1, 2, 4).reshape(B, S, H, dh))
        lses.append(lse.reshape(B, dil, H, L).transpose(0, 3, 1, 2).reshape(B, S, H))
    wts = jax.nn.softmax(jnp.stack(lses), axis=0)
    out = jnp.sum(wts[..., None] * jnp.stack(outs).astype(jnp.float32), axis=0)
    return out.astype(q.dtype)


def causal_attention(q, k, v, scale):
    B, H, S, dq = q.shape
    nq = S // DENSE_QBLK
    qb = q.reshape(B, H, nq, DENSE_QBLK, dq).transpose(2, 0, 1, 3, 4)
    kpos = jnp.arange(S)

    def blk(args):
        qblk, i = args
        qpos = i * DENSE_QBLK + jnp.arange(DENSE_QBLK)
        s = jnp.einsum('bhqd,bhkd->bhqk', qblk, k).astype(jnp.float32) * scale
        s = jnp.where(kpos[None, :] <= qpos[:, None], s, -jnp.inf)
        p = jax.nn.softmax(s, axis=-1).astype(v.dtype)
        return jnp.einsum('bhqk,bhkd->bhqd', p, v)

    o = lax.map(blk, (qb, jnp.arange(nq)))
    return o.transpose(1, 2, 0, 3, 4).reshape(B, H, S, v.shape[-1])


def mla_attention(c_q, c_kv, k_r, g_q, g_kv, w_uq, w_ukv, cos, sin):
    B, S, _ = c_q.shape
    q = (rmsnorm(c_q, g_q) @ w_uq).reshape(B, S, H_MLA, MLA_NOPE + MLA_ROPE)
    q = jnp.concatenate([q[..., :MLA_NOPE], rope(q[..., MLA_NOPE:], cos, sin)], axis=-1)
    kv = (rmsnorm(c_kv, g_kv) @ w_ukv).reshape(B, S, H_MLA, MLA_NOPE + MLA_V)
    k_rope = jnp.broadcast_to(rope(k_r[:, :, None, :], cos, sin), (B, S, H_MLA, MLA_ROPE))
    k = jnp.concatenate([kv[..., :MLA_NOPE], k_rope], axis=-1)
    v = kv[..., MLA_NOPE:]
    o = causal_attention(q.transpose(0, 2, 1, 3), k.transpose(0, 2, 1, 3), v.transpose(0, 2, 1, 3),
                         (MLA_NOPE + MLA_ROPE) ** -0.5)
    return o.transpose(0, 2, 1, 3).reshape(B, S, D_MLA)


def moba_attention(q, k, v, bias_tab):
    B, H, S, dh = q.shape
    nblk = -(-S // MOBA_BLOCK)
    padw = ((0, 0), (0, 0), (0, nblk * MOBA_BLOCK - S), (0, 0))
    kb = jnp.pad(k, padw).reshape(B, H, nblk, MOBA_BLOCK, dh)
    vb = jnp.pad(v, padw).reshape(B, H, nblk, MOBA_BLOCK, dh)
    kmean = jnp.mean(kb.astype(jnp.float32), axis=3)
    topk = min(MOBA_TOPK, nblk)
    nq = S // MOBA_QBLK
    qc = q.reshape(B, H, nq, MOBA_QBLK, dh).transpose(2, 0, 1, 3, 4)
    bi = jnp.arange(B)[:, None, None, None]
    hi = jnp.arange(H)[None, :, None, None]
    hi5 = jnp.arange(H)[None, :, None, None, None]
    bias_h = bias_tab.T
    scale = dh ** -0.5
    offs = jnp.arange(MOBA_BLOCK)

    def blk(args):
        qblk, c = args
        q0 = c * MOBA_QBLK
        qpos = q0 + jnp.arange(MOBA_QBLK)
        j = q0 // MOBA_BLOCK
        gate = jnp.einsum('bhqd,bhnd->bhqn', qblk.astype(jnp.float32), kmean)
        gate = jnp.where(jnp.arange(nblk) < j, gate, -jnp.inf)
        gval, idx = lax.top_k(gate, topk)
        sel_ok = gval > -jnp.inf
        kg = kb[bi, hi, idx]
        vg = vb[bi, hi, idx]
        s_sel = jnp.einsum('bhqd,bhqnkd->bhqnk', qblk, kg).astype(jnp.float32) * scale
        kpos_sel = idx[..., None] * MOBA_BLOCK + offs
        s_sel = s_sel + bias_h[hi5, t5_bucket(qpos[None, None, :, None, None] - kpos_sel)].astype(jnp.float32)
        s_sel = jnp.where(sel_ok[..., None], s_sel, -jnp.inf).reshape(B, H, MOBA_QBLK, topk * MOBA_BLOCK)
        k_own = lax.dynamic_index_in_dim(kb, j, axis=2, keepdims=False)
        v_own = lax.dynamic_index_in_dim(vb, j, axis=2, keepdims=False)
        rel_own = qpos[:, None] - (j * MOBA_BLOCK + offs)[None, :]
        s_own = jnp.einsum('bhqd,bhkd->bhqk', qblk, k_own).astype(jnp.float32) * scale
        s_own = s_own + bias_h[:, t5_bucket(rel_own)][None].astype(jnp.float32)
        s_own = jnp.where(rel_own >= 0, s_own, -jnp.inf)
        p = jax.nn.softmax(jnp.concatenate([s_own, s_sel], axis=-1), axis=-1).astype(v.dtype)
        p_sel = p[..., MOBA_BLOCK:].reshape(B, H, MOBA_QBLK, topk, MOBA_BLOCK)
        return (jnp.einsum('bhqk,bhkd->bhqd', p[..., :MOBA_BLOCK], v_own)
                + jnp.einsum('bhqnk,bhqnkd->bhqd', p_sel, vg))

    o = lax.map(blk, (qc, jnp.arange(nq)))
    return o.transpose(1, 2, 0, 3, 4).reshape(B, H, S, dh)


def setup_inputs(seed: int = 0) -> dict:
    key = jax.random.key(seed)
    ks = jax.random.split(key, 14)
    nrm = jax.random.normal
    f32 = jnp.float32
    return {
        'x': nrm(ks[0], (BATCH, SEQ, D_MODEL), f32),
        'g_attn': 1.0 + 0.05 * nrm(ks[1], (DEPTH, D_MODEL), f32),
        'w_in': nrm(ks[2], (DEPTH, D_MODEL, D_IN), f32) * D_MODEL ** -0.5,
        'g_q_lora': 1.0 + 0.05 * nrm(ks[3], (DEPTH, MLA_Q_LORA), f32),
        'g_kv_lora': 1.0 + 0.05 * nrm(ks[4], (DEPTH, MLA_KV_LORA), f32),
        'w_uq': nrm(ks[5], (DEPTH, MLA_Q_LORA, H_MLA * (MLA_NOPE + MLA_ROPE)), f32) * MLA_Q_LORA ** -0.5,
        'w_ukv': nrm(ks[6], (DEPTH, MLA_KV_LORA, H_MLA * (MLA_NOPE + MLA_V)), f32) * MLA_KV_LORA ** -0.5,
        'rel_bias': 0.5 * nrm(ks[7], (NUM_BUCKETS, H_DIL + H_MOBA), f32),
        'g_mix': 1.0 + 0.05 * nrm(ks[8], (DEPTH, D_MIX), f32),
        'w_o': nrm(ks[9], (DEPTH, D_MIX, D_MODEL), f32) * D_MIX ** -0.5,
        'g_mlp': 1.0 + 0.05 * nrm(ks[10], (DEPTH, D_MODEL), f32),
        'w_up': nrm(ks[11], (DEPTH, D_MODEL, D_FF), f32) * D_MODEL ** -0.5,
        'w_down': nrm(ks[12], (DEPTH, D_FF, D_MODEL), f32) * D_FF ** -0.5,
        'g_final': 1.0 + 0.05 * nrm(ks[13], (D_MODEL,), f32),
    }


def reference(x, g_attn, w_in, g_q_lora, g_kv_lora, w_uq, w_ukv, rel_bias, g_mix, w_o, g_mlp,
              w_up, w_down, g_final):
    B, S, _ = x.shape
    inv_freq = ROPE_THETA ** (-jnp.arange(0, MLA_ROPE, 2, dtype=jnp.float32) / MLA_ROPE)
    ang = jnp.arange(S, dtype=jnp.float32)[:, None] * inv_freq[None, :]
    cos = jnp.cos(ang)[:, None, :]
    sin = jnp.sin(ang)[:, None, :]
    bias_dil = rel_bias[:, :H_DIL]
    bias_moba = rel_bias[:, H_DIL:]
    points = []
    acc = 0
    for size in SPLIT_SIZES[:-1]:
        acc += size
        points.append(acc)
    for l in range(DEPTH):
        h = rmsnorm(x, g_attn[l])
        proj = h @ w_in[l]
        q_a, k_a, v_a, c_q, c_kv, k_r, q_c, k_c, v_c = jnp.split(proj, points, axis=-1)
        o_a = dilated_mixture(q_a.reshape(B, S, H_DIL, HEAD_DIM), k_a.reshape(B, S, H_DIL, HEAD_DIM),
                              v_a.reshape(B, S, H_DIL, HEAD_DIM), bias_dil).reshape(B, S, D_DIL)
        o_b = mla_attention(c_q, c_kv, k_r, g_q_lora[l], g_kv_lora[l], w_uq[l], w_ukv[l], cos, sin)
        o_c = moba_attention(q_c.reshape(B, S, H_MOBA, HEAD_DIM).transpose(0, 2, 1, 3),
                             k_c.reshape(B, S, H_MOBA, HEAD_DIM).transpose(0, 2, 1, 3),
                             v_c.reshape(B, S, H_MOBA, HEAD_DIM).transpose(0, 2, 1, 3),
                             bias_moba).transpose(0, 2, 1, 3).reshape(B, S, D_MOBA)
        gm = g_mix[l]
        mixed = jnp.concatenate([rmsnorm(o_a, gm[:D_DIL]),
                                 rmsnorm(o_b, gm[D_DIL:D_DIL + D_MLA]),
                                 rmsnorm(o_c, gm[D_DIL + D_MLA:])], axis=-1)
        x = x + mixed @ w_o[l]
        h = rmsnorm(x, g_mlp[l])
        x = x + jnp.square(jax.nn.relu(h @ w_up[l])) @ w_down[l]
    return rmsnorm(x, g_final)
```

```python
import math
from contextlib import ExitStack

import numpy as np
import concourse.bass as bass
import concourse.mybir as mybir
from concourse.bass_utils import run_bass_kernel_spmd

F32 = mybir.dt.float32
BF16 = mybir.dt.bfloat16
AF = mybir.ActivationFunctionType
ALU = mybir.AluOpType
AX = mybir.AxisListType

D = 1024
S_LEN = 4096
DEPTH = 4
NCORES = 8
TW = 512
NT = S_LEN // TW
EPS = 1e-6
NEG = -30000.0
H_DIL, H_MLA, H_MOBA = 6, 6, 4
PATTERNS = ((128, 1), (512, 4), (2048, 16))
WIN_COLS = 2048 + 640
MSTRIP_W = 2560
MSTRIP_CLAMP = 2048
GPL = 28
LAUNCH_MODE = 'multi'


class Sched:
    def __init__(self, nc, ndma=(('sp', 16), ('pool', 40))):
        self.nc = nc
        self.eng = {'pe': nc.tensor, 'act': nc.scalar, 'dve': nc.vector, 'pool': nc.gpsimd, 'sp': nc.sync}
        self.sem = {}
        self.val = {}
        self.kidx = {}
        for e in self.eng:
            self.sem[e] = nc.alloc_semaphore('s_' + e)
            self.val[e] = 0
            self.kidx[e] = len(self.kidx)
        self.dq = {}
        for q, n in ndma:
            lst = []
            for i in range(n):
                k = 'd_%s_%d' % (q, i)
                self.sem[k] = nc.alloc_semaphore(k)
                self.val[k] = 0
                self.kidx[k] = len(self.kidx)
                lst.append(k)
            self.dq[q] = [lst, 0]
        nk = len(self.kidx)
        self.vc = {e: np.zeros(nk, np.int64) for e in self.eng}
        self.snap = {}
        self.res = {}
        self.n_ins = 0
        self.n_wait = {e: 0 for e in self.eng}

    def _wait(self, e, key, v):
        ki = self.kidx[key]
        if v <= 0 or self.vc[e][ki] >= v:
            return
        self.eng[e].wait_ge(self.sem[key], v)
        self.n_wait[e] += 1
        sn = self.snap.get((key, v))
        if sn is not None and key != e:
            np.maximum(self.vc[e], sn, out=self.vc[e])
        self.vc[e][ki] = v

    def _deps(self, e, reads, writes, own=None):
        need = {}
        for r in reads:
            st = self.res.get(r)
            if st and st['w']:
                k, v = st['w']
                if k == own and e == 'pe':
                    continue
                need[k] = max(need.get(k, 0), v)
        for w in writes:
            st = self.res.get(w)
            if not st:
                continue
            if st['w'] and st['w'][0] != own:
                k, v = st['w']
                need[k] = max(need.get(k, 0), v)
            for k, v in st['r'].items():
                if k != own:
                    need[k] = max(need.get(k, 0), v)
        for k, v in sorted(need.items(), key=lambda kv: -kv[1]):
            self._wait(e, k, v)

    def _record(self, key, v, reads, writes):
        for r in reads:
            st = self.res.setdefault(r, {'w': None, 'r': {}})
            st['r'][key] = max(st['r'].get(key, 0), v)
        for w in writes:
            self.res[w] = {'w': (key, v), 'r': {}}

    def op(self, e, fn, reads=(), writes=()):
        self._deps(e, reads, writes, own=e)
        ins = fn(self.eng[e])
        self.val[e] += 1
        ins.then_inc(self.sem[e], 1)
        sn = self.vc[e].copy()
        self.snap[(e, self.val[e])] = sn
        self._record(e, self.val[e], reads, writes)
        self.n_ins += 1
        return ins

    def dma(self, q, out, in_, reads=(), writes=()):
        lst, idx = self.dq[q]
        key = lst[idx % len(lst)]
        self.dq[q][1] = idx + 1
        self._wait(q, key, self.val[key])
        self._deps(q, reads, writes)
        ins = self.eng[q].dma_start(out=out, in_=in_)
        self.val[key] += 16
        ins.then_inc(self.sem[key], 16)
        self.snap[(key, self.val[key])] = self.vc[q].copy()
        self._record(key, self.val[key], reads, writes)
        self.n_ins += 1
        return ins

    def barrier(self):
        for e in self.eng:
            for k in self.sem:
                self._wait(e, k, self.val[k])


_UID = [0]


def _uid(name):
    _UID[0] += 1
    return "%s_%d" % (name, _UID[0])


class Rot:
    def __init__(self, items):
        self.items = list(items)
        self.i = 0

    def next(self):
        it = self.items[self.i % len(self.items)]
        self.i += 1
        return it


def _t5_bucket(d):
    n = np.maximum(d, 0)
    nf = np.maximum(n, 1).astype(np.float32)
    large = 16 + (np.log(nf / np.float32(16)) / np.float32(math.log(2048 / 16)) * np.float32(16)).astype(np.int32)
    large = np.minimum(large, 31)
    return np.where(n < 16, n, large)


def _host_consts(rel_bias):
    c = {}
    j = np.arange(128)[:, None]
    u = np.arange(256)[None, :]
    dd = u - j
    valid = (dd >= 0) & (dd <= 128)
    ds = np.full((3, H_DIL, 128, 256), NEG, np.float32)
    for p, (_, dil) in enumerate(PATTERNS):
        bk = _t5_bucket(dd * dil)
        for h in range(H_DIL):
            ds[p, h] = np.where(valid, rel_bias[bk, h], np.float32(NEG))
    c['dstrip'] = np.ascontiguousarray(ds.transpose(1, 2, 0, 3).reshape(H_DIL, 128, 3 * 256))
    u = np.arange(MSTRIP_W)[None, :]
    d = u - 384 - j
    bk = _t5_bucket(d)
    ms = np.zeros((H_MOBA, 128, MSTRIP_W), np.float32)
    for h in range(H_MOBA):
        ms[h] = np.where(d >= 0, rel_bias[bk, H_DIL + h], np.float32(NEG))
    c['mstrip'] = ms
    inv_freq = (np.float32(10000.0) ** (-np.arange(0, 32, 2, dtype=np.float32) / np.float32(32))).astype(np.float32)
    ang = (np.arange(S_LEN, dtype=np.float32)[:, None] * inv_freq[None, :]).astype(np.float32)
    cs, sn = np.cos(ang).astype(np.float32).T, np.sin(ang).astype(np.float32).T
    c['rope'] = np.ascontiguousarray(np.stack([np.concatenate([cs, cs], 0), np.concatenate([-sn, sn], 0)], 0))
    c['ident'] = np.eye(128, dtype=np.float32)
    sel = np.zeros((128, 64), np.float32)
    sel[64, :] = 1.0
    c['sel65'] = sel
    e16 = np.zeros((16, 16, 128), np.float32)
    for n in range(16):
        e16[n, n, :] = 1.0
    c['e16'] = e16.reshape(16, 16 * 128)
    tri = (np.arange(128)[None, :] >= np.arange(128)[:, None]).astype(np.float32)
    c['tri'] = tri
    jj = np.arange(16)[:, None]
    nn = np.arange(16)[None, :]
    vm = np.where(nn < jj, 0.0, -1e30).astype(np.float32)
    va = (nn < jj).astype(np.float32)
    ow = (nn == jj).astype(np.float32)
    c['mtab'] = np.ascontiguousarray(np.broadcast_to(np.stack([vm, va, ow], 0).reshape(1, 3 * 256), (128, 768)))
    return c


def _fm(g):
    return np.ascontiguousarray(g.reshape(-1, 128).T)


def _host_weights(inp):
    w = {}
    w_in = inp['w_in']
    Lr = w_in.shape[0]
    zeros64 = np.zeros((Lr, D, 64), np.float32)
    zeros32 = np.zeros((Lr, D, 32), np.float32)
    kr = w_in[:, :, 1664:1696]
    krs = np.concatenate([kr[:, :, 16:32], kr[:, :, 0:16]], -1)
    fm = np.concatenate([
        w_in[:, :, 0:384], w_in[:, :, 384:768], w_in[:, :, 1152:1536], w_in[:, :, 1536:1664],
        zeros64, kr, zeros32, zeros64, krs, zeros32,
        w_in[:, :, 1696:1952], w_in[:, :, 1952:2208],
        w_in[:, :, 768:1152], w_in[:, :, 2208:2464]], -1)
    assert fm.shape[-1] == WIN_COLS
    w['w_in'] = np.ascontiguousarray(fm.reshape(Lr, 8, 128, WIN_COLS).transpose(0, 2, 1, 3)).reshape(Lr, 128, 8 * WIN_COLS)
    uq = inp['w_uq'].reshape(Lr, 384, 6, 96)
    z = np.zeros((Lr, 384, 6, 64), np.float32)
    uqs = np.concatenate([z, uq[..., 80:96], uq[..., 64:80]], -1)
    uqr = np.stack([uq, uqs], 3).reshape(Lr, 384, 6 * 2 * 96)
    w['w_uq'] = np.ascontiguousarray(uqr.reshape(Lr, 3, 128, 1152).transpose(0, 2, 1, 3)).reshape(Lr, 128, 3 * 1152)
    ukv = inp['w_ukv'].reshape(Lr, 128, 6, 128)
    w['w_ukv'] = np.ascontiguousarray(np.concatenate([ukv[..., 0:64].reshape(Lr, 128, 384),
                                                      ukv[..., 64:128].reshape(Lr, 128, 384)], -1))
    w['w_o'] = np.ascontiguousarray(inp['w_o'].reshape(Lr, 8, 128, 1024).transpose(0, 2, 1, 3)).reshape(Lr, 128, 8192)
    w['w_up'] = np.ascontiguousarray(inp['w_up'].reshape(Lr, 8, 128, 32, 128).transpose(0, 3, 2, 1, 4)).reshape(Lr, 32, 128, 1024)
    w['w_down'] = np.ascontiguousarray(inp['w_down'].reshape(Lr, 32, 128, 8, 128).transpose(0, 3, 2, 1, 4)).reshape(Lr, 8, 128, 4096)
    g = np.zeros((128, GPL * Lr + 8), np.float32)
    for l in range(Lr):
        b = l * GPL
        g[:, b:b + 8] = _fm(inp['g_attn'][l])
        g[:, b + 8:b + 16] = _fm(inp['g_mlp'][l])
        g[:, b + 16:b + 24] = _fm(inp['g_mix'][l])
        g[:, b + 24:b + 27] = _fm(inp['g_q_lora'][l])
        g[:, b + 27:b + 28] = _fm(inp['g_kv_lora'][l])
    g[:, GPL * Lr:] = _fm(inp['g_final'])
    w['gains'] = g
    return w


def build_program(depth=DEPTH, debug=False, phases='PABCO', final_norm=True):
    nc = bass.Bass("TRN2", target_bir_lowering=False)
    S = Sched(nc)
    L = depth

    def din(name, shape, dt=F32):
        return nc.dram_tensor(name, list(shape), dt, kind="ExternalInput").ap()

    def dscr(name, shape, dt):
        return nc.dram_tensor(name, list(shape), dt).ap()

    xT_in = din("xT", [D, S_LEN])
    win_f = din("w_in", [L, 128, 8 * WIN_COLS])
    wuq_f = din("w_uq", [L, 128, 3 * 1152])
    wukv_f = din("w_ukv", [L, 128, 768])
    wo_f = din("w_o", [L, 128, 8192])
    wup_f = din("w_up", [L, 32, 128, 1024])
    wdn_f = din("w_down", [L, 8, 128, 4096])
    gains_d = din("gains", [128, GPL * L + 8])
    dstrip_d = din("dstrip", [H_DIL, 128, 768])
    mstrip_d = din("mstrip", [H_MOBA, 128, MSTRIP_W])
    rope_d = din("rope", [2, 32, S_LEN])
    ident_d = din("ident", [128, 128])
    sel65_d = din("sel65", [128, 64])
    e16_d = din("e16", [16, 2048])
    tri_d = din("tri", [128, 128])
    mtab_d = din("mtab", [128, 768])
    yT = nc.dram_tensor("yT", [D, S_LEN], F32, kind="ExternalOutput").ap()

    win_b = dscr("win_b", [L, 128, 8 * WIN_COLS], BF16)
    wuq_b = dscr("wuq_b", [L, 128, 3 * 1152], BF16)
    wukv_b = dscr("wukv_b", [L, 128, 768], BF16)
    wo_b = dscr("wo_b", [L, 128, 8192], BF16)
    wup_b = dscr("wup_b", [L, 32, 128, 1024], BF16)
    wdn_b = dscr("wdn_b", [L, 8, 128, 4096], BF16)
    XT = dscr("XT", [D, S_LEN], F32)
    QaT = dscr("QaT", [384, S_LEN], BF16)
    KaT = dscr("KaT", [384, S_LEN], BF16)
    Va = dscr("Va", [S_LEN, 6 * 65], BF16)
    QbT = dscr("QbT", [6, 96, S_LEN], BF16)
    KbT = dscr("KbT", [6, 96, S_LEN], BF16)
    Vb = dscr("Vb", [S_LEN, 6 * 65], BF16)
    QcT = dscr("QcT", [256, S_LEN], BF16)
    KcT = dscr("KcT", [256, S_LEN], BF16)
    Vc = dscr("Vc", [S_LEN, 4 * 65], BF16)
    MIXT = dscr("MIXT", [D, S_LEN], F32)
    dexp_s = dscr("dexp_s", [H_DIL, 128, 768], F32)
    mexp_s = dscr("mexp_s", [H_MOBA, 128, MSTRIP_W], F32)
    dbg = {}
    if debug:
        dbg['mix'] = nc.dram_tensor("dbg_mix", [D, S_LEN], F32, kind="ExternalOutput").ap()

    def gsb(name, shape, dt):
        return nc.alloc_sbuf_tensor("sb_" + name, list(shape), dt)

    gains = gsb("gains", [128, GPL * L + 8], F32)
    ones_bf = gsb("ones_bf", [128, 128], BF16)
    ident = gsb("ident", [128, 128], F32)
    sel65 = gsb("sel65", [128, 64], F32)
    e16 = gsb("e16", [16, 2048], BF16)
    tri = gsb("tri", [128, 128], BF16)
    mtab = gsb("mtab", [128, 768], F32)
    epst = gsb("epst", [128, 1], F32)
    kmT = gsb("kmT", [128, 2, 16], BF16)
    kmacc = gsb("kmacc", [128, 2, 16], F32)
    kmh_sb = gsb("kmh_sb", [64, 16], BF16)
    P = [nc.alloc_psum_tensor("P%d" % i, [128, 512], F32) for i in range(8)]
    PN = ["P%d" % i for i in range(8)]

    S.dma('sp', gains[:], gains_d, writes=['gains'])
    S.dma('sp', ident[:], ident_d, writes=['ident'])
    S.dma('sp', sel65[:], sel65_d, writes=['sel65'])
    S.dma('sp', mtab[:], mtab_d, writes=['mtab'])
    S.dma('pool', e16[:], e16_d, writes=['e16'])
    S.dma('pool', tri[:], tri_d, writes=['tri'])
    S.op('pool', lambda e: e.memset(ones_bf[:], 1.0), writes=['ones_bf'])
    S.op('pool', lambda e: e.memset(epst[:], EPS), writes=['epst'])

    def cast_weights(l):
        for src, dst, nm, rows, cols in ((win_f, win_b, 'win', 128, 8 * WIN_COLS), (wuq_f, wuq_b, 'wuq', 128, 3456),
                                         (wukv_f, wukv_b, 'wukv', 128, 768), (wo_f, wo_b, 'wo', 128, 8192)):
            step = 4096
            for c0 in range(0, cols, step):
                c1 = min(cols, c0 + step)
                S.dma('pool', dst[l, :, c0:c1], src[l, :, c0:c1], writes=['%s_b%d_%d' % (nm, l, c0)])
        for j in range(32):
            S.dma('pool', wup_b[l, j], wup_f[l, j], writes=['wup_b%d_%d' % (l, j)])
        for oc in range(8):
            S.dma('pool', wdn_b[l, oc], wdn_f[l, oc], writes=['wdn_b%d_%d' % (l, oc)])

    def wres(nm, l, cols, step=4096):
        return ['%s_b%d_%d' % (nm, l, c0) for c0 in range(0, cols, step)]

    cast_weights(0)

    with ExitStack() as es:
        tmp = es.enter_context(nc.sbuf_tensor("su_tmp", [128, MSTRIP_W], F32))
        for h in range(H_DIL):
            S.dma('sp', tmp[:, 0:768], dstrip_d[h], writes=['su_tmp'])
            S.op('act', lambda e: e.activation(out=tmp[:, 0:768], in_=tmp[:, 0:768], func=AF.Exp), reads=['su_tmp'], writes=['su_tmp'])
            S.dma('pool', dexp_s[h], tmp[:, 0:768], reads=['su_tmp'])
        for h in range(H_MOBA):
            S.dma('sp', tmp[:], mstrip_d[h], writes=['su_tmp'])
            S.op('act', lambda e: e.activation(out=tmp[:], in_=tmp[:], func=AF.Exp), reads=['su_tmp'], writes=['su_tmp'])
            S.dma('pool', mexp_s[h], tmp[:], reads=['su_tmp'])
    S.barrier()

    def rstd_from(out_sb, in_ps, inv_n, rd, wr):
        npart = out_sb.shape[0]
        S.op('act', lambda e: e.activation(out=out_sb, in_=in_ps, func=AF.Sqrt, bias=epst[0:npart, :], scale=inv_n),
             reads=rd + ['epst'], writes=wr)
        S.op('dve', lambda e: e.reciprocal(out=out_sb, in_=out_sb), reads=wr, writes=wr)

    def phase_P(l):
        gb = l * GPL
        src = xT_in if l == 0 else XT
        srcv = src.rearrange("(c p) s -> p c s", p=128)
        with ExitStack() as es:
            def sb(name, shape, dt):
                return es.enter_context(nc.sbuf_tensor(_uid("pp_" + name), list(shape), dt))
            win = sb("win", [128, 8, WIN_COLS], BF16)
            wuq = sb("wuq", [128, 3, 1152], BF16)
            wukv = sb("wukv", [128, 768], BF16)
            xt = [sb("xt%d" % i, [128, 8, TW], F32) for i in range(2)]
            sq = sb("sq", [128, 8, TW], BF16)
            xg = sb("xg", [128, 8, TW], BF16)
            rbc = sb("rbc", [128, TW], F32)
            rcol = sb("rcol", [128, 4], F32)
            stg = [sb("stg%d" % i, [128, TW], BF16) for i in range(4)]
            cq = sb("cq", [128, 3, TW], F32)
            cqs = sb("cqs", [128, 3, TW], BF16)
            cqg = sb("cqg", [128, 3, TW], BF16)
            rqbc = sb("rqbc", [128, TW], F32)
            ckv = sb("ckv", [128, TW], F32)
            ckvs = sb("ckvs", [128, TW], BF16)
            ckvg = sb("ckvg", [128, TW], BF16)
            rkbc = sb("rkbc", [128, TW], F32)
            rkcol = sb("rkcol", [128, 4], F32)
            t1 = sb("t1", [128, TW], F32)
            t2 = sb("t2", [128, TW], F32)
            cst = sb("cst", [128, 2, TW], F32)
            vas = [sb("vas%d" % i, [128, 6, 65], BF16) for i in range(2)]
            vbs = [sb("vbs%d" % i, [128, 6, 65], BF16) for i in range(2)]
            vcs = [sb("vcs%d" % i, [128, 4, 65], BF16) for i in range(2)]
            for i in range(2):
                S.op('pool', lambda e: e.memset(vas[i][:], 1.0), writes=['vas%d' % i])
                S.op('pool', lambda e: e.memset(vbs[i][:], 1.0), writes=['vbs%d' % i])
                S.op('pool', lambda e: e.memset(vcs[i][:], 1.0), writes=['vcs%d' % i])
            S.op('pool', lambda e: e.memset(kmacc[:], 0.0), writes=['kmacc'])
            winv = win_b[l].rearrange("p (c n) -> p c n", c=8)
            for c in range(8):
                rs = [r for r in wres('win', l, 8 * WIN_COLS)]
                S.dma('sp', win[:, c, :], winv[:, c, :], reads=rs, writes=['win%d' % c])
            S.dma('sp', wuq[:], wuq_b[l].rearrange("p (c n) -> p c n", c=3), reads=wres('wuq', l, 3456), writes=['wuq'])
            S.dma('sp', wukv[:], wukv_b[l], reads=wres('wukv', l, 768), writes=['wukv'])
            frot = Rot([2, 3, 4])
            srot = Rot(range(4))
            for t in range(NT):
                ts = slice(t * TW, (t + 1) * TW)
                x_ = xt[t % 2]
                xn = 'xt%d' % (t % 2)
                S.dma('sp', x_[:], srcv[:, :, ts], writes=[xn])
                S.dma('sp', cst[64:96, :, :], rope_d[:, :, ts].rearrange("a p s -> p a s"), writes=['cst'])
                S.op('act', lambda e: e.activation(out=sq[:], in_=x_[:], func=AF.Square), reads=[xn], writes=['sq'])
                for c in range(8):
                    S.op('dve', lambda e: e.tensor_scalar(out=xg[:, c, :], in0=x_[:, c, :], scalar1=gains[:, gb + c:gb + c + 1],
                                                          scalar2=None, op0=ALU.mult), reads=[xn, 'gains'], writes=['xg'])
                for c in range(8):
                    S.op('pe', lambda e: e.matmul(P[0][:], lhsT=ones_bf[:], rhs=sq[:, c, :], start=(c == 0), stop=(c == 7)),
                         reads=['ones_bf', 'sq'], writes=['P0'])
                rstd_from(rbc[:], P[0][:], 1.0 / D, ['P0'], ['rbc'])
                for j in range(4):
                    for c in range(8):
                        S.op('pe', lambda e: e.matmul(P[1][:, j:j + 1], lhsT=sq[:, c, j * 128:(j + 1) * 128], rhs=ones_bf[:, 0:1],
                                                      start=(c == 0), stop=(c == 7)), reads=['ones_bf', 'sq'], writes=['P1'])
                rstd_from(rcol[:], P[1][:, 0:4], 1.0 / D, ['P1'], ['rcol'])

                def fm_chunk(j):
                    b = frot.next()
                    for c in range(8):
                        S.op('pe', lambda e: e.matmul(P[b][:], lhsT=win[:, c, j * 128:(j + 1) * 128], rhs=xg[:, c, :],
                                                      start=(c == 0), stop=(c == 7)), reads=['win%d' % c, 'xg'], writes=[PN[b]])
                    return b

                def evac_bc(out_ap, b, bc, bcn, wr, np_=slice(0, 128)):
                    S.op('dve', lambda e: e.tensor_tensor(out=out_ap, in0=P[b][np_, :], in1=bc[np_, :], op=ALU.mult),
                         reads=[PN[b], bcn], writes=wr)

                for j in range(16):
                    if j in (10, 11):
                        continue
                    b = fm_chunk(j)
                    if j < 6 or j >= 12:
                        si = srot.next()
                        evac_bc(stg[si][:], b, rbc, 'rbc', ['stg%d' % si])
                        if j < 3:
                            dst, dn = QaT[j * 128:(j + 1) * 128, ts], 'QaT'
                        elif j < 6:
                            dst, dn = KaT[(j - 3) * 128:(j - 2) * 128, ts], 'KaT'
                        elif j < 14:
                            dst, dn = QcT[(j - 12) * 128:(j - 11) * 128, ts], 'QcT'
                        else:
                            dst, dn = KcT[(j - 14) * 128:(j - 13) * 128, ts], 'KcT'
                            S.op('dve', lambda e: e.tensor_reduce(out=kmacc[:, j - 14, 2 * t:2 * t + 2],
                                                                  in_=stg[si][:].rearrange("p (b k) -> p b k", k=256),
                                                                  axis=AX.X, op=ALU.add), reads=['stg%d' % si], writes=['kmacc'])
                        S.dma('pool', dst, stg[si][:], reads=['stg%d' % si])
                    elif j < 9:
                        evac_bc(cq[:, j - 6, :], b, rbc, 'rbc', ['cq'])
                    else:
                        evac_bc(ckv[:], b, rbc, 'rbc', ['ckv'])
                b10 = fm_chunk(10)
                b11 = fm_chunk(11)
                r = slice(64, 96)
                S.op('dve', lambda e: e.tensor_tensor(out=t1[r, :], in0=P[b10][r, :], in1=cst[r, 0, :], op=ALU.mult),
                     reads=[PN[b10], 'cst'], writes=['t1'])
                S.op('dve', lambda e: e.tensor_tensor(out=t2[r, :], in0=P[b11][r, :], in1=cst[r, 1, :], op=ALU.mult),
                     reads=[PN[b11], 'cst'], writes=['t2'])
                S.op('dve', lambda e: e.tensor_tensor(out=t1[r, :], in0=t1[r, :], in1=t2[r, :], op=ALU.add),
                     reads=['t1', 't2'], writes=['t1'])
                si = srot.next()
                S.op('dve', lambda e: e.tensor_tensor(out=stg[si][r, :], in0=t1[r, :], in1=rbc[r, :], op=ALU.mult),
                     reads=['t1', 'rbc'], writes=['stg%d' % si])
                for h in range(6):
                    S.dma('pool', KbT[h, 64:96, ts], stg[si][r, :], reads=['stg%d' % si])
                for j in range(4):
                    js = slice(j * 128, (j + 1) * 128)
                    for c in range(8):
                        S.op('pe', lambda e: e.matmul(P[5][:, 0:384], lhsT=xg[:, c, js], rhs=win[:, c, 2048:2432],
                                                      start=(c == 0), stop=(c == 7)), reads=['xg', 'win%d' % c], writes=['P5'])
                    for c in range(8):
                        S.op('pe', lambda e: e.matmul(P[6][:, 0:256], lhsT=xg[:, c, js], rhs=win[:, c, 2432:2688],
                                                      start=(c == 0), stop=(c == 7)), reads=['xg', 'win%d' % c], writes=['P6'])
                    va_, vc_ = vas[j % 2], vcs[j % 2]
                    S.op('act', lambda e: e.activation(out=va_[:, :, 0:64], in_=P[5][:, 0:384].rearrange("p (h e) -> p h e", e=64),
                                                       func=AF.Copy, scale=rcol[:, j:j + 1]),
                         reads=['P5', 'rcol'], writes=['vas%d' % (j % 2)])
                    S.op('act', lambda e: e.activation(out=vc_[:, :, 0:64], in_=P[6][:, 0:256].rearrange("p (h e) -> p h e", e=64),
                                                       func=AF.Copy, scale=rcol[:, j:j + 1]),
                         reads=['P6', 'rcol'], writes=['vcs%d' % (j % 2)])
                    rows = slice(t * TW + j * 128, t * TW + (j + 1) * 128)
                    S.dma('pool', Va[rows, :], va_[:].rearrange("p h e -> p (h e)"), reads=['vas%d' % (j % 2)])
                    S.dma('pool', Vc[rows, :], vc_[:].rearrange("p h e -> p (h e)"), reads=['vcs%d' % (j % 2)])
                S.op('act', lambda e: e.activation(out=cqs[:], in_=cq[:], func=AF.Square), reads=['cq'], writes=['cqs'])
                for c in range(3):
                    S.op('pe', lambda e: e.matmul(P[0][:], lhsT=ones_bf[:], rhs=cqs[:, c, :], start=(c == 0), stop=(c == 2)),
                         reads=['ones_bf', 'cqs'], writes=['P0'])
                rstd_from(rqbc[:], P[0][:], 1.0 / 384, ['P0'], ['rqbc'])
                for c in range(3):
                    S.op('dve', lambda e: e.tensor_scalar(out=cqg[:, c, :], in0=cq[:, c, :], scalar1=gains[:, gb + 24 + c:gb + 25 + c],
                                                          scalar2=None, op0=ALU.mult), reads=['cq', 'gains'], writes=['cqg'])
                for h in range(6):
                    b1 = frot.next()
                    for c in range(3):
                        S.op('pe', lambda e: e.matmul(P[b1][0:96, :], lhsT=wuq[:, c, h * 192:h * 192 + 96], rhs=cqg[:, c, :],
                                                      start=(c == 0), stop=(c == 2)), reads=['wuq', 'cqg'], writes=[PN[b1]])
                    for c in range(3):
                        S.op('pe', lambda e: e.matmul(P[7][0:96, :], lhsT=wuq[:, c, h * 192 + 96:h * 192 + 192], rhs=cqg[:, c, :],
                                                      start=(c == 0), stop=(c == 2)), reads=['wuq', 'cqg'], writes=['P7'])
                    si = srot.next()
                    evac_bc(stg[si][0:64, :], b1, rqbc, 'rqbc', ['stg%d' % si], slice(0, 64))
                    S.op('dve', lambda e: e.tensor_tensor(out=t1[r, :], in0=P[b1][r, :], in1=cst[r, 0, :], op=ALU.mult),
                         reads=[PN[b1], 'cst'], writes=['t1'])
                    S.op('dve', lambda e: e.tensor_tensor(out=t2[r, :], in0=P[7][r, :], in1=cst[r, 1, :], op=ALU.mult),
                         reads=['P7', 'cst'], writes=['t2'])
                    S.op('dve', lambda e: e.tensor_tensor(out=t1[r, :], in0=t1[r, :], in1=t2[r, :], op=ALU.add),
                         reads=['t1', 't2'], writes=['t1'])
                    S.op('dve', lambda e: e.tensor_tensor(out=stg[si][r, :], in0=t1[r, :], in1=rqbc[r, :], op=ALU.mult),
                         reads=['t1', 'rqbc'], writes=['stg%d' % si])
                    S.dma('pool', QbT[h, :, ts], stg[si][0:96, :], reads=['stg%d' % si])
                S.op('act', lambda e: e.activation(out=ckvs[:], in_=ckv[:], func=AF.Square), reads=['ckv'], writes=['ckvs'])
                S.op('pe', lambda e: e.matmul(P[0][:], lhsT=ones_bf[:], rhs=ckvs[:], start=True, stop=True),
                     reads=['ones_bf', 'ckvs'], writes=['P0'])
                rstd_from(rkbc[:], P[0][:], 1.0 / 128, ['P0'], ['rkbc'])
                for j in range(4):
                    S.op('pe', lambda e: e.matmul(P[1][:, j:j + 1], lhsT=ckvs[:, j * 128:(j + 1) * 128], rhs=ones_bf[:, 0:1],
                                                  start=True, stop=True), reads=['ones_bf', 'ckvs'], writes=['P1'])
                rstd_from(rkcol[:], P[1][:, 0:4], 1.0 / 128, ['P1'], ['rkcol'])
                S.op('dve', lambda e: e.tensor_scalar(out=ckvg[:], in0=ckv[:], scalar1=gains[:, gb + 27:gb + 28], scalar2=None,
                                                      op0=ALU.mult), reads=['ckv', 'gains'], writes=['ckvg'])
                for hp in range(3):
                    b = frot.next()
                    S.op('pe', lambda e: e.matmul(P[b][:], lhsT=wukv[:, hp * 128:(hp + 1) * 128], rhs=ckvg[:], start=True, stop=True),
                         reads=['wukv', 'ckvg'], writes=[PN[b]])
                    si = srot.next()
                    evac_bc(stg[si][:], b, rkbc, 'rkbc', ['stg%d' % si])
                    S.dma('pool', KbT[2 * hp, 0:64, ts], stg[si][0:64, :], reads=['stg%d' % si])
                    S.dma('pool', KbT[2 * hp + 1, 0:64, ts], stg[si][64:128, :], reads=['stg%d' % si])
                for j in range(4):
                    js = slice(j * 128, (j + 1) * 128)
                    S.op('pe', lambda e: e.matmul(P[5][:, 0:384], lhsT=ckvg[:, js], rhs=wukv[:, 384:768], start=True, stop=True),
                         reads=['ckvg', 'wukv'], writes=['P5'])
                    vb_ = vbs[j % 2]
                    S.op('act', lambda e: e.activation(out=vb_[:, :, 0:64], in_=P[5][:, 0:384].rearrange("p (h e) -> p h e", e=64),
                                                       func=AF.Copy, scale=rkcol[:, j:j + 1]),
                         reads=['P5', 'rkcol'], writes=['vbs%d' % (j % 2)])
                    rows = slice(t * TW + j * 128, t * TW + (j + 1) * 128)
                    S.dma('pool', Vb[rows, :], vb_[:].rearrange("p h e -> p (h e)"), reads=['vbs%d' % (j % 2)])
            S.op('dve', lambda e: e.tensor_scalar(out=kmT[:], in0=kmacc[:], scalar1=1.0 / 256, scalar2=None, op0=ALU.mult),
                 reads=['kmacc'], writes=['kmT'])
            S.barrier()

    def finalize_head(oT_sb, oT_name, row0, es_sb, tag):
        zr, osg = es_sb
        for t in range(NT):
            ts = slice(t * TW, (t + 1) * TW)
            S.op('pe', lambda e: e.matmul(P[7][0:64, :], lhsT=sel65[0:65, :], rhs=oT_sb[0:65, ts], start=True, stop=True),
                 reads=['sel65', oT_name], writes=['P7'])
            S.op('dve', lambda e: e.reciprocal(out=zr[0:64, :], in_=P[7][0:64, :]), reads=['P7'], writes=[tag + 'zr'])
            o_ = osg[t % 2]
            S.op('dve', lambda e: e.tensor_tensor(out=o_[0:64, :], in0=oT_sb[0:64, ts], in1=zr[0:64, :], op=ALU.mult),
                 reads=[oT_name, tag + 'zr'], writes=[tag + 'osg%d' % (t % 2)])
            S.dma('pool', MIXT[row0:row0 + 64, ts], o_[0:64, :], reads=[tag + 'osg%d' % (t % 2)])

    def phase_A(l):
        with ExitStack() as es:
            def sb(name, shape, dt):
                return es.enter_context(nc.sbuf_tensor(_uid("pa_" + name), list(shape), dt))
            qT = sb("qT", [64, S_LEN], BF16)
            kT = sb("kT", [64, S_LEN], BF16)
            qd = [None, sb("qd4", [64, S_LEN], BF16), sb("qd16", [64, S_LEN], BF16)]
            kd = [None, sb("kd4", [64, S_LEN], BF16), sb("kd16", [64, S_LEN], BF16)]
            qd[0], kd[0] = qT, kT
            qn = ['pa_qT', 'pa_qd4', 'pa_qd16']
            kn = ['pa_kT', 'pa_kd4', 'pa_kd16']
            vv = [sb("v%d" % p, [128, 32, 65], BF16) for p in range(3)]
            dex = sb("dex", [128, 3, 256], F32)
            oT = sb("oT", [65, S_LEN], F32)
            E = [sb("E%d" % i, [128, 256], BF16) for i in range(3)]
            Pm = [sb("Pm%d" % i, [128, 256], BF16) for i in range(3)]
            zr = sb("zr", [64, TW], F32)
            osg = [sb("osg%d" % i, [64, TW], F32) for i in range(2)]
            for h in range(H_DIL):
                S.dma('sp', qT[:], QaT[h * 64:(h + 1) * 64, :], writes=['pa_qT'])
                S.dma('sp', kT[:], KaT[h * 64:(h + 1) * 64, :], writes=['pa_kT'])
                S.dma('sp', dex[:], dexp_s[h].rearrange("p (a u) -> p a u", a=3), writes=['pa_dex'])
                S.dma('sp', vv[0][:], Va.rearrange("(kt p) (h e) -> p kt h e", p=128, e=65)[:, :, h, :], writes=['pa_v0_0'])
                for p in (1, 2):
                    dil = PATTERNS[p][1]
                    nA = S_LEN // dil // 128
                    vsrc = Va.rearrange("(a j r) (h e) -> j r a h e", j=128, r=dil, e=65)
                    for r in range(dil):
                        S.dma('sp', vv[p][:, r * nA:(r + 1) * nA, :], vsrc[:, r, :, h, :], writes=['pa_v%d_%d' % (p, r)])
                    S.op('pool', lambda e: e.tensor_copy(out=qd[p][:].rearrange("p (r m) -> p r m", r=dil),
                                                         in_=qT[:].rearrange("p (m r) -> p r m", r=dil)),
                         reads=['pa_qT'], writes=[qn[p]])
                    S.op('pool', lambda e: e.tensor_copy(out=kd[p][:].rearrange("p (r m) -> p r m", r=dil),
                                                         in_=kT[:].rearrange("p (m r) -> p r m", r=dil)),
                         reads=['pa_kT'], writes=[kn[p]])
                S.op('pool', lambda e: e.memset(oT[:], 0.0), writes=['pa_oT'])
                tiles = []
                for p in range(3):
                    dil = PATTERNS[p][1]
                    nA = S_LEN // dil // 128
                    for kt in range(32):
                        a = kt % nA
                        nq = 256 if a + 1 < nA else 128
                        tiles.append((p, dil, kt // nA, a, kt, nq))
                srot = Rot([0, 1, 2])
                orot = Rot([3, 4, 5])
                pend = None

                def qk(i):
                    p, dil, r, a, kt, nq = tiles[i]
                    b = srot.next()
                    S.op('pe', lambda e: e.matmul(P[b][:, 0:nq], lhsT=kd[p][:, kt * 128:(kt + 1) * 128],
                                                  rhs=qd[p][:, kt * 128:kt * 128 + nq], start=True, stop=True),
                         reads=[kn[p], qn[p]], writes=[PN[b]])
                    ei = i % 3
                    S.op('act', lambda e: e.activation(out=E[ei][:, 0:nq], in_=P[b][:, 0:nq], func=AF.Exp, scale=0.125),
                         reads=[PN[b]], writes=['pa_E%d' % ei])
                    S.op('dve', lambda e: e.tensor_tensor(out=Pm[ei][:, 0:nq], in0=E[ei][:, 0:nq], in1=dex[:, p, 0:nq], op=ALU.mult),
                         reads=['pa_E%d' % ei, 'pa_dex'], writes=['pa_Pm%d' % ei])

                def pv(i):
                    p, dil, r, a, kt, nq = tiles[i]
                    ei = i % 3
                    b = orot.next()
                    S.op('pe', lambda e: e.matmul(P[b][0:65, 0:nq], lhsT=vv[p][:, kt, :], rhs=Pm[ei][:, 0:nq], start=True, stop=True),
                         reads=['pa_v%d_%d' % (p, r), 'pa_Pm%d' % ei], writes=[PN[b]])
                    ov = oT[:].rearrange("p (m r) -> p r m", r=dil)[:, r, a * 128:a * 128 + nq]
                    S.op('dve', lambda e: e.tensor_tensor(out=ov, in0=P[b][0:65, 0:nq], in1=ov, op=ALU.add),
                         reads=[PN[b], 'pa_oT'], writes=['pa_oT'])

                for i in range(len(tiles)):
                    qk(i)
                    if i >= 1:
                        pv(i - 1)
                pv(len(tiles) - 1)
                finalize_head(oT, 'pa_oT', h * 64, (zr, osg), 'pa_')
            S.barrier()

    def causal_phase(l, kind):
        moba = kind == 'c'
        nh = H_MOBA if moba else H_MLA
        dq = 64 if moba else 96
        scale = 0.125 if moba else 96 ** -0.5
        with ExitStack() as es:
            def sb(name, shape, dt):
                return es.enter_context(nc.sbuf_tensor(_uid("pc_" + name), list(shape), dt))
            qT = sb("qT", [dq, S_LEN], BF16)
            kT = sb("kT", [dq, S_LEN], BF16)
            v = sb("v", [128, 32, 65], BF16)
            E = [sb("E%d" % i, [128, TW], BF16) for i in range(3)]
            oS = sb("oS", [65, S_LEN], F32)
            zr = sb("zr", [64, TW], F32)
            osg = [sb("osg%d" % i, [64, TW], F32) for i in range(2)]
            if moba:
                mex = sb("mex", [128, MSTRIP_W], F32)
                Pm = [sb("Pm%d" % i, [128, TW], BF16) for i in range(3)]
                negT = sb("negT", [16, S_LEN], BF16)
                gm = sb("gm", [128, 16], F32)
                top8 = sb("top8", [128, 8], F32)
                selq = sb("selq", [128, 16], F32)
            for h in range(nh):
                if moba:
                    S.dma('sp', qT[:], QcT[h * 64:(h + 1) * 64, :], writes=['pc_qT'])
                    S.dma('sp', kT[:], KcT[h * 64:(h + 1) * 64, :], writes=['pc_kT'])
                    S.dma('sp', v[:], Vc.rearrange("(kt p) (h e) -> p kt h e", p=128, e=65)[:, :, h, :], writes=['pc_v'])
                    S.dma('sp', mex[:], mexp_s[h], writes=['pc_mex'])
                    S.dma('sp', kmh_sb[:], kmT[(h % 2) * 64:(h % 2) * 64 + 64, h // 2, :], reads=['kmT'], writes=['pc_kmh'])
                    for i in range(32):
                        j = i // 2
                        S.op('pe', lambda e: e.matmul(P[6][:, 0:16], lhsT=qT[0:64, i * 128:(i + 1) * 128], rhs=kmh_sb[:],
                                                      start=True, stop=True), reads=['pc_qT', 'pc_kmh'], writes=['P6'])
                        S.op('dve', lambda e: e.tensor_tensor(out=gm[:], in0=P[6][:, 0:16], in1=mtab[:, j * 16:(j + 1) * 16], op=ALU.add),
                             reads=['P6', 'mtab'], writes=['pc_gm'])
                        S.op('dve', lambda e: e.max(out=top8[:], in_=gm[:]), reads=['pc_gm'], writes=['pc_top8'])
                        S.op('dve', lambda e: e.tensor_scalar(out=selq[:], in0=gm[:], scalar1=top8[:, 2:3], scalar2=None, op0=ALU.is_ge),
                             reads=['pc_gm', 'pc_top8'], writes=['pc_selq'])
                        S.op('dve', lambda e: e.tensor_tensor(out=selq[:], in0=selq[:], in1=mtab[:, 256 + j * 16:256 + (j + 1) * 16], op=ALU.mult),
                             reads=['pc_selq', 'mtab'], writes=['pc_selq'])
                        S.op('dve', lambda e: e.tensor_tensor(out=selq[:], in0=selq[:], in1=mtab[:, 512 + j * 16:512 + (j + 1) * 16], op=ALU.add),
                             reads=['pc_selq', 'mtab'], writes=['pc_selq'])
                        S.op('dve', lambda e: e.tensor_scalar(out=selq[:], in0=selq[:], scalar1=-1.0, scalar2=-NEG, op0=ALU.add, op1=ALU.mult),
                             reads=['pc_selq'], writes=['pc_selq'])
                        S.op('pe', lambda e: e.transpose(out=P[7][0:16, 0:128], in_=selq[:], identity=ident[:]),
                             reads=['pc_selq', 'ident'], writes=['P7'])
                        S.op('act', lambda e: e.activation(out=negT[:, i * 128:(i + 1) * 128], in_=P[7][0:16, 0:128], func=AF.Copy),
                             reads=['P7'], writes=['pc_negT'])
                else:
                    S.dma('sp', qT[:], QbT[h], writes=['pc_qT'])
                    S.dma('sp', kT[:], KbT[h], writes=['pc_kT'])
                    S.dma('sp', v[:], Vb.rearrange("(kt p) (h e) -> p kt h e", p=128, e=65)[:, :, h, :], writes=['pc_v'])
                tiles = []
                for qg in range(NT):
                    nk = 4 * qg + 4
                    for kt in range(nk):
                        c = max(0, kt - 4 * qg)
                        tiles.append((qg, kt, c, kt == 0, kt == nk - 1))
                srot = Rot([0, 1, 2])
                orot = Rot([3, 4])
                ob = [None]

                def qk(i):
                    qg, kt, c, first, last = tiles[i]
                    c0 = c * 128
                    nq = TW - c0
                    q0 = qg * TW + c0
                    b = srot.next()
                    S.op('pe', lambda e: e.matmul(P[b][:, 0:nq], lhsT=kT[:, kt * 128:(kt + 1) * 128], rhs=qT[:, q0:q0 + nq],
                                                  start=True, stop=not moba), reads=['pc_kT', 'pc_qT'], writes=[PN[b]])
                    if moba:
                        n = kt // 2
                        S.op('pe', lambda e: e.matmul(P[b][:, 0:nq], lhsT=e16[:, n * 128:(n + 1) * 128], rhs=negT[:, q0:q0 + nq],
                                                      start=False, stop=True), reads=['e16', 'pc_negT'], writes=[PN[b]])
                    ei = i % 3
                    S.op('act', lambda e: e.activation(out=E[ei][:, 0:nq], in_=P[b][:, 0:nq], func=AF.Exp, scale=scale),
                         reads=[PN[b]], writes=['pc_E%d' % ei])
                    if moba:
                        u0 = min(q0 - kt * 128 + 384, MSTRIP_CLAMP)
                        S.op('dve', lambda e: e.tensor_tensor(out=Pm[ei][:, 0:nq], in0=E[ei][:, 0:nq], in1=mex[:, u0:u0 + nq], op=ALU.mult),
                             reads=['pc_E%d' % ei, 'pc_mex'], writes=['pc_Pm%d' % ei])
                    elif kt >= 4 * qg:
                        S.op('dve', lambda e: e.tensor_tensor(out=E[ei][:, 0:128], in0=E[ei][:, 0:128], in1=tri[:], op=ALU.mult),
                             reads=['pc_E%d' % ei, 'tri'], writes=['pc_E%d' % ei])

                def pv(i):
                    qg, kt, c, first, last = tiles[i]
                    c0 = c * 128
                    nq = TW - c0
                    ei = i % 3
                    if first:
                        ob[0] = orot.next()
                    b = ob[0]
                    src_, sn = (Pm[ei], 'pc_Pm%d' % ei) if moba else (E[ei], 'pc_E%d' % ei)
                    S.op('pe', lambda e: e.matmul(P[b][0:65, c0:TW], lhsT=v[:, kt, :], rhs=src_[:, 0:nq], start=first, stop=last),
                         reads=['pc_v', sn], writes=[PN[b]])
                    if last:
                        S.op('act', lambda e: e.activation(out=oS[:, qg * TW:(qg + 1) * TW], in_=P[b][0:65, :], func=AF.Copy),
                             reads=[PN[b]], writes=['pc_oS'])

                for i in range(len(tiles)):
                    qk(i)
                    if i >= 1:
                        pv(i - 1)
                pv(len(tiles) - 1)
                row0 = (768 if moba else 384) + h * 64
                finalize_head(oS, 'pc_oS', row0, (zr, osg), 'pc_')
            S.barrier()

    def phase_O(l, last_layer):
        gb = l * GPL
        src = xT_in if l == 0 else XT
        srcv = src.rearrange("(c p) s -> p c s", p=128)
        dstv = XT.rearrange("(c p) s -> p c s", p=128)
        mixv = MIXT.rearrange("(c p) s -> p c s", p=128)
        yv = yT.rearrange("(c p) s -> p c s", p=128)
        with ExitStack() as es:
            def sb(name, shape, dt):
                return es.enter_context(nc.sbuf_tensor(_uid("po_" + name), list(shape), dt))
            wo = sb("wo", [128, 8, 1024], BF16)
            wup = [sb("wup%d" % i, [128, 4, 8, 128], BF16) for i in range(2)]
            wdn = [sb("wdn%d" % i, [128, 32, 128], BF16) for i in range(2)]
            xt = sb("xt", [128, 8, TW], F32)
            mx = sb("mx", [128, 8, TW], F32)
            sq = sb("sq", [128, 8, TW], BF16)
            mn = sb("mn", [128, 8, TW], BF16)
            rg = [sb("rg%d" % i, [128, TW], F32) for i in range(3)]
            x1 = sb("x1", [128, 8, TW], F32)
            hg = sb("hg", [128, 8, TW], BF16)
            rbc = sb("rbc", [128, TW], F32)
            r2 = sb("r2", [128, TW], F32)
            rr = sb("rr", [128, TW], BF16)
            u = sb("u", [128, 32, TW], BF16)
            yo = sb("yo", [128, 8, TW], F32)
            S.dma('sp', wo[:], wo_b[l].rearrange("p (c n) -> p c n", c=8), reads=wres('wo', l, 8192), writes=['po_wo'])
            prot = Rot([1, 2, 3, 4, 5, 6, 7])
            groups = ((0, 3), (3, 6), (6, 8))
            for t in range(NT):
                ts = slice(t * TW, (t + 1) * TW)
                S.dma('sp', xt[:], srcv[:, :, ts], writes=['po_xt'])
                S.dma('sp', mx[:], mixv[:, :, ts], writes=['po_mx'])
                S.op('act', lambda e: e.activation(out=sq[:], in_=mx[:], func=AF.Square), reads=['po_mx'], writes=['po_sq'])
                for gi, (c0, c1) in enumerate(groups):
                    for c in range(c0, c1):
                        S.op('pe', lambda e: e.matmul(P[0][:], lhsT=ones_bf[:], rhs=sq[:, c, :], start=(c == c0), stop=(c == c1 - 1)),
                             reads=['ones_bf', 'po_sq'], writes=['P0'])
                    rstd_from(rg[gi][:], P[0][:], 1.0 / ((c1 - c0) * 128), ['P0'], ['po_rg%d' % gi])
                    for c in range(c0, c1):
                        S.op('dve', lambda e: e.scalar_tensor_tensor(out=mn[:, c, :], in0=mx[:, c, :], scalar=gains[:, gb + 16 + c:gb + 17 + c],
                                                                     in1=rg[gi][:], op0=ALU.mult, op1=ALU.mult),
                             reads=['po_mx', 'gains', 'po_rg%d' % gi], writes=['po_mn'])
                for oc in range(8):
                    b = prot.next()
                    for c in range(8):
                        S.op('pe', lambda e: e.matmul(P[b][:], lhsT=wo[:, c, oc * 128:(oc + 1) * 128], rhs=mn[:, c, :],
                                                      start=(c == 0), stop=(c == 7)), reads=['po_wo', 'po_mn'], writes=[PN[b]])
                    S.op('dve', lambda e: e.tensor_tensor(out=x1[:, oc, :], in0=P[b][:], in1=xt[:, oc, :], op=ALU.add),
                         reads=[PN[b], 'po_xt'], writes=['po_x1'])
                S.op('act', lambda e: e.activation(out=sq[:], in_=x1[:], func=AF.Square), reads=['po_x1'], writes=['po_sq'])
                for c in range(8):
                    S.op('pe', lambda e: e.matmul(P[0][:], lhsT=ones_bf[:], rhs=sq[:, c, :], start=(c == 0), stop=(c == 7)),
                         reads=['ones_bf', 'po_sq'], writes=['P0'])
                rstd_from(rbc[:], P[0][:], 1.0 / D, ['P0'], ['po_rbc'])
                S.op('pool', lambda e: e.tensor_tensor(out=r2[:], in0=rbc[:], in1=rbc[:], op=ALU.mult), reads=['po_rbc'], writes=['po_r2'])
                for c in range(8):
                    S.op('dve', lambda e: e.tensor_scalar(out=hg[:, c, :], in0=x1[:, c, :], scalar1=gains[:, gb + 8 + c:gb + 9 + c],
                                                          scalar2=None, op0=ALU.mult), reads=['po_x1', 'gains'], writes=['po_hg'])
                for jg in range(8):
                    wb_ = wup[jg % 2]
                    wn = 'po_wup%d' % (jg % 2)
                    S.dma('sp', wb_[:], wup_b[l, jg * 4:(jg + 1) * 4].rearrange("j p (c m) -> p j c m", c=8),
                          reads=['wup_b%d_%d' % (l, jg * 4 + q) for q in range(4)], writes=[wn])
                    for jj in range(4):
                        j = jg * 4 + jj
                        b = prot.next()
                        for c in range(8):
                            S.op('pe', lambda e: e.matmul(P[b][:], lhsT=wb_[:, jj, c, :], rhs=hg[:, c, :], start=(c == 0), stop=(c == 7)),
                                 reads=[wn, 'po_hg'], writes=[PN[b]])
                        S.op('act', lambda e: e.activation(out=rr[:], in_=P[b][:], func=AF.Relu), reads=[PN[b]], writes=['po_rr'])
                        S.op('dve', lambda e: e.tensor_tensor(out=u[:, j, :], in0=rr[:], in1=rr[:], op=ALU.mult),
                             reads=['po_rr'], writes=['po_u'])
                for oc in range(8):
                    wb_ = wdn[oc % 2]
                    wn = 'po_wdn%d' % (oc % 2)
                    S.dma('sp', wb_[:], wdn_b[l, oc].rearrange("p (k m) -> p k m", k=32), reads=['wdn_b%d_%d' % (l, oc)], writes=[wn])
                    b = prot.next()
                    for k in range(32):
                        S.op('pe', lambda e: e.matmul(P[b][:], lhsT=wb_[:, k, :], rhs=u[:, k, :], start=(k == 0), stop=(k == 31)),
                             reads=[wn, 'po_u'], writes=[PN[b]])
                    S.op('dve', lambda e: e.tensor_tensor(out=yo[:, oc, :], in0=P[b][:], in1=r2[:], op=ALU.mult),
                         reads=[PN[b], 'po_r2'], writes=['po_yo'])
                    S.op('pool', lambda e: e.tensor_tensor(out=yo[:, oc, :], in0=yo[:, oc, :], in1=x1[:, oc, :], op=ALU.add),
                         reads=['po_yo', 'po_x1'], writes=['po_yo'])
                if not last_layer:
                    S.dma('pool', dstv[:, :, ts], yo[:], reads=['po_yo'])
                elif not final_norm:
                    S.dma('pool', yv[:, :, ts], yo[:], reads=['po_yo'])
                else:
                    gf = GPL * L
                    S.op('act', lambda e: e.activation(out=sq[:], in_=yo[:], func=AF.Square), reads=['po_yo'], writes=['po_sq'])
                    for c in range(8):
                        S.op('pe', lambda e: e.matmul(P[0][:], lhsT=ones_bf[:], rhs=sq[:, c, :], start=(c == 0), stop=(c == 7)),
                             reads=['ones_bf', 'po_sq'], writes=['P0'])
                    rstd_from(rbc[:], P[0][:], 1.0 / D, ['P0'], ['po_rbc'])
                    for c in range(8):
                        S.op('dve', lambda e: e.scalar_tensor_tensor(out=x1[:, c, :], in0=yo[:, c, :], scalar=gains[:, gf + c:gf + c + 1],
                                                                     in1=rbc[:], op0=ALU.mult, op1=ALU.mult),
                             reads=['po_yo', 'gains', 'po_rbc'], writes=['po_x1'])
                    S.dma('pool', yv[:, :, ts], x1[:], reads=['po_x1'])
            S.barrier()

    for l in range(L):
        if 'P' in phases:
            phase_P(l)
        if l + 1 < L:
            cast_weights(l + 1)
        if 'A' in phases:
            phase_A(l)
        if 'B' in phases:
            causal_phase(l, 'b')
        if 'C' in phases:
            causal_phase(l, 'c')
        if debug and l == 0:
            S.dma('sp', dbg['mix'], MIXT)
            S.barrier()
        if 'O' in phases:
            phase_O(l, l == L - 1)
    S.barrier()
    return nc, S


_HOST_INPUT_NAMES = ("w_in", "w_uq", "w_ukv", "w_o", "w_up", "w_down", "gains",
                     "dstrip", "mstrip", "rope", "ident", "sel65", "e16", "tri", "mtab")


def kernel(x, g_attn, w_in, g_q_lora, g_kv_lora, w_uq, w_ukv, rel_bias, g_mix, w_o, g_mlp, w_up, w_down, g_final,
           _depth=DEPTH, _debug=False):
    inp = dict(g_attn=np.asarray(g_attn), w_in=np.asarray(w_in), g_q_lora=np.asarray(g_q_lora), g_kv_lora=np.asarray(g_kv_lora),
               w_uq=np.asarray(w_uq), w_ukv=np.asarray(w_ukv), g_mix=np.asarray(g_mix), w_o=np.asarray(w_o),
               g_mlp=np.asarray(g_mlp), w_up=np.asarray(w_up), w_down=np.asarray(w_down), g_final=np.asarray(g_final))
    if _depth != DEPTH:
        for k in list(inp):
            if k != 'g_final':
                inp[k] = inp[k][:_depth]
    x = np.asarray(x)
    rel_bias = np.asarray(rel_bias, dtype=np.float32)
    if LAUNCH_MODE == 'multi' and not _debug:
        consts = _host_consts(rel_bias)
        progs = {}
        cur = [np.ascontiguousarray(x[c].T) for c in range(NCORES)]
        for l in range(_depth):
            last = l == _depth - 1
            if last not in progs:
                _UID[0] = 0
                progs[last] = build_program(1, False, 'PABCO', final_norm=last)[0]
            li = {k: (v if k == 'g_final' else v[l:l + 1]) for k, v in inp.items()}
            shared = _host_weights(li)
            shared.update(consts)
            in_maps = []
            for c in range(NCORES):
                m = {k: shared[k] for k in _HOST_INPUT_NAMES}
                m["xT"] = cur[c]
                in_maps.append(m)
            res = run_bass_kernel_spmd(progs[last], in_maps, core_ids=list(range(NCORES)))
            cur = [np.ascontiguousarray(res.results[c]["yT"]) for c in range(NCORES)]
        return np.stack([np.ascontiguousarray(cur[c].T) for c in range(NCORES)], 0).astype(np.float32)
    shared = _host_weights(inp)
    shared.update(_host_consts(rel_bias))
    nc, _ = build_program(_depth, _debug)
    in_maps = []
    for c in range(NCORES):
        m = {k: shared[k] for k in _HOST_INPUT_NAMES}
        m["xT"] = np.ascontiguousarray(x[c].T)
        in_maps.append(m)
    res = run_bass_kernel_spmd(nc, in_maps, core_ids=list(range(NCORES)))
    out = np.stack([np.ascontiguousarray(res.results[c]["yT"].T) for c in range(NCORES)], 0).astype(np.float32)
    if _debug:
        return out, res
    return out
```

```python
import math
from contextlib import ExitStack

import numpy as np
import concourse.bass as bass
import concourse.mybir as mybir
from concourse.bass_utils import run_bass_kernel_spmd

F32 = mybir.dt.float32
BF16 = mybir.dt.bfloat16
AF = mybir.ActivationFunctionType
ALU = mybir.AluOpType
AX = mybir.AxisListType

D = 1024
S_LEN = 4096
DEPTH = 4
NCORES = 8
TW = 512
NT = S_LEN // TW
EPS = 1e-6
NEG = -30000.0
H_DIL, H_MLA, H_MOBA = 6, 6, 4
PATTERNS = ((128, 1), (512, 4), (2048, 16))
WIN_COLS = 2048 + 640
MSTRIP_W = 2560
MSTRIP_CLAMP = 2048
GPL = 28
LAUNCH_MODE = 'multi'


class Sched:
    def __init__(self, nc, ndma=(('sp', 16), ('pool', 40))):
        self.nc = nc
        self.eng = {'pe': nc.tensor, 'act': nc.scalar, 'dve': nc.vector, 'pool': nc.gpsimd, 'sp': nc.sync}
        self.sem = {}
        self.val = {}
        self.kidx = {}
        for e in self.eng:
            self.sem[e] = nc.alloc_semaphore('s_' + e)
            self.val[e] = 0
            self.kidx[e] = len(self.kidx)
        self.dq = {}
        for q, n in ndma:
            lst = []
            for i in range(n):
                k = 'd_%s_%d' % (q, i)
                self.sem[k] = nc.alloc_semaphore(k)
                self.val[k] = 0
                self.kidx[k] = len(self.kidx)
                lst.append(k)
            self.dq[q] = [lst, 0]
        nk = len(self.kidx)
        self.vc = {e: np.zeros(nk, np.int64) for e in self.eng}
        self.snap = {}
        self.res = {}
        self.n_ins = 0
        self.n_wait = {e: 0 for e in self.eng}

    def _wait(self, e, key, v):
        ki = self.kidx[key]
        if v <= 0 or self.vc[e][ki] >= v:
            return
        self.eng[e].wait_ge(self.sem[key], v)
        self.n_wait[e] += 1
        sn = self.snap.get((key, v))
        if sn is not None and key != e:
            np.maximum(self.vc[e], sn, out=self.vc[e])
        self.vc[e][ki] = v

    def _deps(self, e, reads, writes, own=None):
        need = {}
        for r in reads:
            st = self.res.get(r)
            if st and st['w']:
                k, v = st['w']
                if k == own and e == 'pe':
                    continue
                need[k] = max(need.get(k, 0), v)
        for w in writes:
            st = self.res.get(w)
            if not st:
                continue
            if st['w'] and st['w'][0] != own:
                k, v = st['w']
                need[k] = max(need.get(k, 0), v)
            for k, v in st['r'].items():
                if k != own:
                    need[k] = max(need.get(k, 0), v)
        for k, v in sorted(need.items(), key=lambda kv: -kv[1]):
            self._wait(e, k, v)

    def _record(self, key, v, reads, writes):
        for r in reads:
            st = self.res.setdefault(r, {'w': None, 'r': {}})
            st['r'][key] = max(st['r'].get(key, 0), v)
        for w in writes:
            self.res[w] = {'w': (key, v), 'r': {}}

    def op(self, e, fn, reads=(), writes=()):
        self._deps(e, reads, writes, own=e)
        ins = fn(self.eng[e])
        self.val[e] += 1
        ins.then_inc(self.sem[e], 1)
        sn = self.vc[e].copy()
        self.snap[(e, self.val[e])] = sn
        self._record(e, self.val[e], reads, writes)
        self.n_ins += 1
        return ins

    def dma(self, q, out, in_, reads=(), writes=()):
        lst, idx = self.dq[q]
        key = lst[idx % len(lst)]
        self.dq[q][1] = idx + 1
        self._wait(q, key, self.val[key])
        self._deps(q, reads, writes)
        ins = self.eng[q].dma_start(out=out, in_=in_)
        self.val[key] += 16
        ins.then_inc(self.sem[key], 16)
        self.snap[(key, self.val[key])] = self.vc[q].copy()
        self._record(key, self.val[key], reads, writes)
        self.n_ins += 1
        return ins

    def barrier(self):
        for e in self.eng:
            for k in self.sem:
                self._wait(e, k, self.val[k])


_UID = [0]


def _uid(name):
    _UID[0] += 1
    return "%s_%d" % (name, _UID[0])


class Rot:
    def __init__(self, items):
        self.items = list(items)
        self.i = 0

    def next(self):
        it = self.items[self.i % len(self.items)]
        self.i += 1
        return it


def _t5_bucket(d):
    n = np.maximum(d, 0)
    nf = np.maximum(n, 1).astype(np.float32)
    large = 16 + (np.log(nf / np.float32(16)) / np.float32(math.log(2048 / 16)) * np.float32(16)).astype(np.int32)
    large = np.minimum(large, 31)
    return np.where(n < 16, n, large)


def _host_consts(rel_bias):
    c = {}
    j = np.arange(128)[:, None]
    u = np.arange(256)[None, :]
    dd = u - j
    valid = (dd >= 0) & (dd <= 128)
    ds = np.full((3, H_DIL, 128, 256), NEG, np.float32)
    for p, (_, dil) in enumerate(PATTERNS):
        bk = _t5_bucket(dd * dil)
        for h in range(H_DIL):
            ds[p, h] = np.where(valid, rel_bias[bk, h], np.float32(NEG))
    c['dstrip'] = np.ascontiguousarray(ds.transpose(1, 2, 0, 3).reshape(H_DIL, 128, 3 * 256))
    u = np.arange(MSTRIP_W)[None, :]
    d = u - 384 - j
    bk = _t5_bucket(d)
    ms = np.zeros((H_MOBA, 128, MSTRIP_W), np.float32)
    for h in range(H_MOBA):
        ms[h] = np.where(d >= 0, rel_bias[bk, H_DIL + h], np.float32(NEG))
    c['mstrip'] = ms
    inv_freq = (np.float32(10000.0) ** (-np.arange(0, 32, 2, dtype=np.float32) / np.float32(32))).astype(np.float32)
    ang = (np.arange(S_LEN, dtype=np.float32)[:, None] * inv_freq[None, :]).astype(np.float32)
    cs, sn = np.cos(ang).astype(np.float32).T, np.sin(ang).astype(np.float32).T
    c['rope'] = np.ascontiguousarray(np.stack([np.concatenate([cs, cs], 0), np.concatenate([-sn, sn], 0)], 0))
    c['ident'] = np.eye(128, dtype=np.float32)
    sel = np.zeros((128, 64), np.float32)
    sel[64, :] = 1.0
    c['sel65'] = sel
    e16 = np.zeros((16, 16, 128), np.float32)
    for n in range(16):
        e16[n, n, :] = 1.0
    c['e16'] = e16.reshape(16, 16 * 128)
    tri = (np.arange(128)[None, :] >= np.arange(128)[:, None]).astype(np.float32)
    c['tri'] = tri
    jj = np.arange(16)[:, None]
    nn = np.arange(16)[None, :]
    vm = np.where(nn < jj, 0.0, -1e30).astype(np.float32)
    va = (nn < jj).astype(np.float32)
    ow = (nn == jj).astype(np.float32)
    c['mtab'] = np.ascontiguousarray(np.broadcast_to(np.stack([vm, va, ow], 0).reshape(1, 3 * 256), (128, 768)))
    return c


def _fm(g):
    return np.ascontiguousarray(g.reshape(-1, 128).T)


def _host_weights(inp):
    w = {}
    w_in = inp['w_in']
    Lr = w_in.shape[0]
    zeros64 = np.zeros((Lr, D, 64), np.float32)
    zeros32 = np.zeros((Lr, D, 32), np.float32)
    kr = w_in[:, :, 1664:1696]
    krs = np.concatenate([kr[:, :, 16:32], kr[:, :, 0:16]], -1)
    fm = np.concatenate([
        w_in[:, :, 0:384], w_in[:, :, 384:768], w_in[:, :, 1152:1536], w_in[:, :, 1536:1664],
        zeros64, kr, zeros32, zeros64, krs, zeros32,
        w_in[:, :, 1696:1952], w_in[:, :, 1952:2208],
        w_in[:, :, 768:1152], w_in[:, :, 2208:2464]], -1)
    assert fm.shape[-1] == WIN_COLS
    w['w_in'] = np.ascontiguousarray(fm.reshape(Lr, 8, 128, WIN_COLS).transpose(0, 2, 1, 3)).reshape(Lr, 128, 8 * WIN_COLS)
    uq = inp['w_uq'].reshape(Lr, 384, 6, 96)
    z = np.zeros((Lr, 384, 6, 64), np.float32)
    uqs = np.concatenate([z, uq[..., 80:96], uq[..., 64:80]], -1)
    uqr = np.stack([uq, uqs], 3).reshape(Lr, 384, 6 * 2 * 96)
    w['w_uq'] = np.ascontiguousarray(uqr.reshape(Lr, 3, 128, 1152).transpose(0, 2, 1, 3)).reshape(Lr, 128, 3 * 1152)
    ukv = inp['w_ukv'].reshape(Lr, 128, 6, 128)
    w['w_ukv'] = np.ascontiguousarray(np.concatenate([ukv[..., 0:64].reshape(Lr, 128, 384),
                                                      ukv[..., 64:128].reshape(Lr, 128, 384)], -1))
    w['w_o'] = np.ascontiguousarray(inp['w_o'].reshape(Lr, 8, 128, 1024).transpose(0, 2, 1, 3)).reshape(Lr, 128, 8192)
    w['w_up'] = np.ascontiguousarray(inp['w_up'].reshape(Lr, 8, 128, 32, 128).transpose(0, 3, 2, 1, 4)).reshape(Lr, 32, 128, 1024)
    w['w_down'] = np.ascontiguousarray(inp['w_down'].reshape(Lr, 32, 128, 8, 128).transpose(0, 3, 2, 1, 4)).reshape(Lr, 8, 128, 4096)
    g = np.zeros((128, GPL * Lr + 8), np.float32)
    for l in range(Lr):
        b = l * GPL
        g[:, b:b + 8] = _fm(inp['g_attn'][l])
        g[:, b + 8:b + 16] = _fm(inp['g_mlp'][l])
        g[:, b + 16:b + 24] = _fm(inp['g_mix'][l])
        g[:, b + 24:b + 27] = _fm(inp['g_q_lora'][l])
        g[:, b + 27:b + 28] = _fm(inp['g_kv_lora'][l])
    g[:, GPL * Lr:] = _fm(inp['g_final'])
    w['gains'] = g
    return w


def build_program(depth=DEPTH, debug=False, phases='PABCO', final_norm=True):
    nc = bass.Bass("TRN2", target_bir_lowering=False)
    S = Sched(nc)
    L = depth

    def din(name, shape, dt=F32):
        return nc.dram_tensor(name, list(shape), dt, kind="ExternalInput").ap()

    def dscr(name, shape, dt):
        return nc.dram_tensor(name, list(shape), dt).ap()

    xT_in = din("xT", [D, S_LEN])
    win_f = din("w_in", [L, 128, 8 * WIN_COLS])
    wuq_f = din("w_uq", [L, 128, 3 * 1152])
    wukv_f = din("w_ukv", [L, 128, 768])
    wo_f = din("w_o", [L, 128, 8192])
    wup_f = din("w_up", [L, 32, 128, 1024])
    wdn_f = din("w_down", [L, 8, 128, 4096])
    gains_d = din("gains", [128, GPL * L + 8])
    dstrip_d = din("dstrip", [H_DIL, 128, 768])
    mstrip_d = din("mstrip", [H_MOBA, 128, MSTRIP_W])
    rope_d = din("rope", [2, 32, S_LEN])
    ident_d = din("ident", [128, 128])
    sel65_d = din("sel65", [128, 64])
    e16_d = din("e16", [16, 2048])
    tri_d = din("tri", [128, 128])
    mtab_d = din("mtab", [128, 768])
    yT = nc.dram_tensor("yT", [D, S_LEN], F32, kind="ExternalOutput").ap()

    win_b = dscr("win_b", [L, 128, 8 * WIN_COLS], BF16)
    wuq_b = dscr("wuq_b", [L, 128, 3 * 1152], BF16)
    wukv_b = dscr("wukv_b", [L, 128, 768], BF16)
    wo_b = dscr("wo_b", [L, 128, 8192], BF16)
    wup_b = dscr("wup_b", [L, 32, 128, 1024], BF16)
    wdn_b = dscr("wdn_b", [L, 8, 128, 4096], BF16)
    XT = dscr("XT", [D, S_LEN], F32)
    QaT = dscr("QaT", [384, S_LEN], BF16)
    KaT = dscr("KaT", [384, S_LEN], BF16)
    Va = dscr("Va", [S_LEN, 6 * 65], BF16)
    QbT = dscr("QbT", [6, 96, S_LEN], BF16)
    KbT = dscr("KbT", [6, 96, S_LEN], BF16)
    Vb = dscr("Vb", [S_LEN, 6 * 65], BF16)
    QcT = dscr("QcT", [256, S_LEN], BF16)
    KcT = dscr("KcT", [256, S_LEN], BF16)
    Vc = dscr("Vc", [S_LEN, 4 * 65], BF16)
    MIXT = dscr("MIXT", [D, S_LEN], F32)
    dexp_s = dscr("dexp_s", [H_DIL, 128, 768], F32)
    mexp_s = dscr("mexp_s", [H_MOBA, 128, MSTRIP_W], F32)
    dbg = {}
    if debug:
        dbg['mix'] = nc.dram_tensor("dbg_mix", [D, S_LEN], F32, kind="ExternalOutput").ap()

    def gsb(name, shape, dt):
        return nc.alloc_sbuf_tensor("sb_" + name, list(shape), dt)

    gains = gsb("gains", [128, GPL * L + 8], F32)
    ones_bf = gsb("ones_bf", [128, 128], BF16)
    ident = gsb("ident", [128, 128], F32)
    sel65 = gsb("sel65", [128, 64], F32)
    e16 = gsb("e16", [16, 2048], BF16)
    tri = gsb("tri", [128, 128], BF16)
    mtab = gsb("mtab", [128, 768], F32)
    epst = gsb("epst", [128, 1], F32)
    kmT = gsb("kmT", [128, 2, 16], BF16)
    kmacc = gsb("kmacc", [128, 2, 16], F32)
    kmh_sb = gsb("kmh_sb", [64, 16], BF16)
    P = [nc.alloc_psum_tensor("P%d" % i, [128, 512], F32) for i in range(8)]
    PN = ["P%d" % i for i in range(8)]

    S.dma('sp', gains[:], gains_d, writes=['gains'])
    S.dma('sp', ident[:], ident_d, writes=['ident'])
    S.dma('sp', sel65[:], sel65_d, writes=['sel65'])
    S.dma('sp', mtab[:], mtab_d, writes=['mtab'])
    S.dma('pool', e16[:], e16_d, writes=['e16'])
    S.dma('pool', tri[:], tri_d, writes=['tri'])
    S.op('pool', lambda e: e.memset(ones_bf[:], 1.0), writes=['ones_bf'])
    S.op('pool', lambda e: e.memset(epst[:], EPS), writes=['epst'])

    def cast_weights(l):
        for src, dst, nm, rows, cols in ((win_f, win_b, 'win', 128, 8 * WIN_COLS), (wuq_f, wuq_b, 'wuq', 128, 3456),
                                         (wukv_f, wukv_b, 'wukv', 128, 768), (wo_f, wo_b, 'wo', 128, 8192)):
            step = 4096
            for c0 in range(0, cols, step):
                c1 = min(cols, c0 + step)
                S.dma('pool', dst[l, :, c0:c1], src[l, :, c0:c1], writes=['%s_b%d_%d' % (nm, l, c0)])
        for j in range(32):
            S.dma('pool', wup_b[l, j], wup_f[l, j], writes=['wup_b%d_%d' % (l, j)])
        for oc in range(8):
            S.dma('pool', wdn_b[l, oc], wdn_f[l, oc], writes=['wdn_b%d_%d' % (l, oc)])

    def wres(nm, l, cols, step=4096):
        return ['%s_b%d_%d' % (nm, l, c0) for c0 in range(0, cols, step)]

    cast_weights(0)

    with ExitStack() as es:
        tmp = es.enter_context(nc.sbuf_tensor("su_tmp", [128, MSTRIP_W], F32))
        for h in range(H_DIL):
            S.dma('sp', tmp[:, 0:768], dstrip_d[h], writes=['su_tmp'])
            S.op('act', lambda e: e.activation(out=tmp[:, 0:768], in_=tmp[:, 0:768], func=AF.Exp), reads=['su_tmp'], writes=['su_tmp'])
            S.dma('pool', dexp_s[h], tmp[:, 0:768], reads=['su_tmp'])
        for h in range(H_MOBA):
            S.dma('sp', tmp[:], mstrip_d[h], writes=['su_tmp'])
            S.op('act', lambda e: e.activation(out=tmp[:], in_=tmp[:], func=AF.Exp), reads=['su_tmp'], writes=['su_tmp'])
            S.dma('pool', mexp_s[h], tmp[:], reads=['su_tmp'])
    S.barrier()

    def rstd_from(out_sb, in_ps, inv_n, rd, wr):
        npart = out_sb.shape[0]
        S.op('act', lambda e: e.activation(out=out_sb, in_=in_ps, func=AF.Ln, bias=epst[0:npart, :], scale=inv_n),
             reads=rd + ['epst'], writes=wr)
        S.op('act', lambda e: e.activation(out=out_sb, in_=out_sb, func=AF.Exp, scale=-0.5), reads=wr, writes=wr)

    def phase_P(l):
        gb = l * GPL
        src = xT_in if l == 0 else XT
        srcv = src.rearrange("(c p) s -> p c s", p=128)
        with ExitStack() as es:
            def sb(name, shape, dt):
                return es.enter_context(nc.sbuf_tensor(_uid("pp_" + name), list(shape), dt))
            win = sb("win", [128, 8, WIN_COLS], BF16)
            wuq = sb("wuq", [128, 3, 1152], BF16)
            wukv = sb("wukv", [128, 768], BF16)
            xt = [sb("xt%d" % i, [128, 8, TW], F32) for i in range(2)]
            cst = [sb("cst%d" % i, [128, 2, TW], F32) for i in range(2)]
            sq = sb("sq", [128, 8, TW], BF16)
            xg = sb("xg", [128, 8, TW], BF16)
            rbc = sb("rbc", [128, TW], F32)
            rcol = sb("rcol", [128, 4], F32)
            sQa = [sb("sQa%d" % i, [128, 3, TW], BF16) for i in range(2)]
            sKa = [sb("sKa%d" % i, [128, 3, TW], BF16) for i in range(2)]
            sQc = [sb("sQc%d" % i, [128, 2, TW], BF16) for i in range(2)]
            sKc = [sb("sKc%d" % i, [128, 2, TW], BF16) for i in range(2)]
            sQb = [sb("sQb%d" % i, [96, 6, TW], BF16) for i in range(2)]
            sKb = [sb("sKb%d" % i, [128, 3, TW], BF16) for i in range(2)]
            sKr = [sb("sKr%d" % i, [96, 6, TW], BF16) for i in range(2)]
            sVa = [sb("sVa%d" % i, [128, 4, 6, 65], BF16) for i in range(2)]
            sVb = [sb("sVb%d" % i, [128, 4, 6, 65], BF16) for i in range(2)]
            sVc = [sb("sVc%d" % i, [128, 4, 4, 65], BF16) for i in range(2)]
            cq = sb("cq", [128, 3, TW], F32)
            cqs = sb("cqs", [128, 3, TW], BF16)
            cqg = sb("cqg", [128, 3, TW], BF16)
            rqbc = sb("rqbc", [128, TW], F32)
            ckv = sb("ckv", [128, TW], F32)
            ckvs = sb("ckvs", [128, TW], BF16)
            ckvg = sb("ckvg", [128, TW], BF16)
            rkbc = sb("rkbc", [128, TW], F32)
            rkcol = sb("rkcol", [128, 4], F32)
            t1 = sb("t1", [128, TW], F32)
            t2 = sb("t2", [128, TW], F32)
            for i in range(2):
                S.op('pool', lambda e: e.memset(sVa[i][:], 1.0), writes=['sVa%d' % i])
                S.op('pool', lambda e: e.memset(sVb[i][:], 1.0), writes=['sVb%d' % i])
                S.op('pool', lambda e: e.memset(sVc[i][:], 1.0), writes=['sVc%d' % i])
            S.op('pool', lambda e: e.memset(kmacc[:], 0.0), writes=['kmacc'])
            winv = win_b[l].rearrange("p (c n) -> p c n", c=8)
            for c in range(8):
                S.dma('sp', win[:, c, :], winv[:, c, :], reads=wres('win', l, 8 * WIN_COLS), writes=['win%d' % c])
            S.dma('sp', wuq[:], wuq_b[l].rearrange("p (c n) -> p c n", c=3), reads=wres('wuq', l, 3456), writes=['wuq'])
            S.dma('sp', wukv[:], wukv_b[l], reads=wres('wukv', l, 768), writes=['wukv'])
            frot = Rot([2, 3, 4])
            r = slice(64, 96)

            def load_x(t):
                ts_ = slice(t * TW, (t + 1) * TW)
                S.dma('sp', xt[t % 2][:], srcv[:, :, ts_], writes=['xt%d' % (t % 2)])
                S.dma('sp', cst[t % 2][64:96, :, :], rope_d[:, :, ts_].rearrange("a p s -> p a s"), writes=['cst%d' % (t % 2)])

            load_x(0)
            for t in range(NT):
                ts = slice(t * TW, (t + 1) * TW)
                tb = t % 2
                x_, xn = xt[tb], 'xt%d' % tb
                cs_, cn = cst[tb], 'cst%d' % tb
                if t + 1 < NT:
                    load_x(t + 1)
                S.op('act', lambda e: e.activation(out=sq[:], in_=x_[:], func=AF.Square), reads=[xn], writes=['sq'])
                for c in range(8):
                    S.op('dve', lambda e: e.tensor_scalar(out=xg[:, c, :], in0=x_[:, c, :], scalar1=gains[:, gb + c:gb + c + 1],
                                                          scalar2=None, op0=ALU.mult), reads=[xn, 'gains'], writes=['xg'])
                for c in range(8):
                    S.op('pe', lambda e: e.matmul(P[0][:], lhsT=ones_bf[:], rhs=sq[:, c, :], start=(c == 0), stop=(c == 7)),
                         reads=['ones_bf', 'sq'], writes=['P0'])
                rstd_from(rbc[:], P[0][:], 1.0 / D, ['P0'], ['rbc'])
                for j in range(4):
                    for c in range(8):
                        S.op('pe', lambda e: e.matmul(P[1][:, j:j + 1], lhsT=sq[:, c, j * 128:(j + 1) * 128], rhs=ones_bf[:, 0:1],
                                                      start=(c == 0), stop=(c == 7)), reads=['ones_bf', 'sq'], writes=['P1'])
                rstd_from(rcol[:], P[1][:, 0:4], 1.0 / D, ['P1'], ['rcol'])

                def fm_chunk(j):
                    b = frot.next()
                    for c in range(8):
                        S.op('pe', lambda e: e.matmul(P[b][:], lhsT=win[:, c, j * 128:(j + 1) * 128], rhs=xg[:, c, :],
                                                      start=(c == 0), stop=(c == 7)), reads=['win%d' % c, 'xg'], writes=[PN[b]])
                    return b

                def evac_bc(out_ap, b, bc, bcn, wr, np_=slice(0, 128)):
                    S.op('dve', lambda e: e.tensor_tensor(out=out_ap, in0=P[b][np_, :], in1=bc[np_, :], op=ALU.mult),
                         reads=[PN[b], bcn], writes=wr)

                def store_fm(dram, stage, sname):
                    S.dma('pool', dram.rearrange("(c p) s -> p c s", p=128)[:, :, ts], stage[:], reads=[sname])

                for j in range(16):
                    if j in (10, 11):
                        continue
                    b = fm_chunk(j)
                    if j < 3:
                        evac_bc(sQa[tb][:, j, :], b, rbc, 'rbc', ['sQa%d' % tb])
                        if j == 2:
                            store_fm(QaT, sQa[tb], 'sQa%d' % tb)
                    elif j < 6:
                        evac_bc(sKa[tb][:, j - 3, :], b, rbc, 'rbc', ['sKa%d' % tb])
                        if j == 5:
                            store_fm(KaT, sKa[tb], 'sKa%d' % tb)
                    elif j < 9:
                        evac_bc(cq[:, j - 6, :], b, rbc, 'rbc', ['cq'])
                    elif j == 9:
                        evac_bc(ckv[:], b, rbc, 'rbc', ['ckv'])
                    elif j < 14:
                        evac_bc(sQc[tb][:, j - 12, :], b, rbc, 'rbc', ['sQc%d' % tb])
                        if j == 13:
                            store_fm(QcT, sQc[tb], 'sQc%d' % tb)
                    else:
                        evac_bc(sKc[tb][:, j - 14, :], b, rbc, 'rbc', ['sKc%d' % tb])
                        S.op('dve', lambda e: e.tensor_reduce(out=kmacc[:, j - 14, 2 * t:2 * t + 2],
                                                              in_=sKc[tb][:, j - 14, :].rearrange("p (b k) -> p b k", k=256),
                                                              axis=AX.X, op=ALU.add), reads=['sKc%d' % tb], writes=['kmacc'])
                        if j == 15:
                            store_fm(KcT, sKc[tb], 'sKc%d' % tb)
                b10 = fm_chunk(10)
                b11 = fm_chunk(11)
                S.op('dve', lambda e: e.tensor_tensor(out=t1[r, :], in0=P[b10][r, :], in1=cs_[r, 0, :], op=ALU.mult),
                     reads=[PN[b10], cn], writes=['t1'])
                S.op('dve', lambda e: e.tensor_tensor(out=t2[r, :], in0=P[b11][r, :], in1=cs_[r, 1, :], op=ALU.mult),
                     reads=[PN[b11], cn], writes=['t2'])
                S.op('dve', lambda e: e.tensor_tensor(out=t1[r, :], in0=t1[r, :], in1=t2[r, :], op=ALU.add),
                     reads=['t1', 't2'], writes=['t1'])
                S.op('dve', lambda e: e.tensor_tensor(out=sKr[tb][r, 0, :], in0=t1[r, :], in1=rbc[r, :], op=ALU.mult),
                     reads=['t1', 'rbc'], writes=['sKr%d' % tb])
                for h in range(1, 6):
                    S.op('pool', lambda e: e.tensor_copy(out=sKr[tb][r, h, :], in_=sKr[tb][r, 0, :]), reads=['sKr%d' % tb], writes=['sKr%d_%d' % (tb, h)])
                S.dma('pool', KbT[:, 64:96, ts].rearrange("h p s -> p h s"), sKr[tb][r, :, :],
                      reads=['sKr%d' % tb] + ['sKr%d_%d' % (tb, h) for h in range(1, 6)])
                for j in range(4):
                    js = slice(j * 128, (j + 1) * 128)
                    for c in range(8):
                        S.op('pe', lambda e: e.matmul(P[5][:, 0:384], lhsT=xg[:, c, js], rhs=win[:, c, 2048:2432],
                                                      start=(c == 0), stop=(c == 7)), reads=['xg', 'win%d' % c], writes=['P5'])
                    for c in range(8):
                        S.op('pe', lambda e: e.matmul(P[6][:, 0:256], lhsT=xg[:, c, js], rhs=win[:, c, 2432:2688],
                                                      start=(c == 0), stop=(c == 7)), reads=['xg', 'win%d' % c], writes=['P6'])
                    S.op('act', lambda e: e.activation(out=sVa[tb][:, j, :, 0:64], in_=P[5][:, 0:384].rearrange("p (h e) -> p h e", e=64),
                                                       func=AF.Copy, scale=rcol[:, j:j + 1]),
                         reads=['P5', 'rcol'], writes=['sVa%d' % tb])
                    S.op('act', lambda e: e.activation(out=sVc[tb][:, j, :, 0:64], in_=P[6][:, 0:256].rearrange("p (h e) -> p h e", e=64),
                                                       func=AF.Copy, scale=rcol[:, j:j + 1]),
                         reads=['P6', 'rcol'], writes=['sVc%d' % tb])
                S.dma('pool', Va.rearrange("(t j p) c -> t p j c", j=4, p=128)[t], sVa[tb][:].rearrange("p j h e -> p j (h e)"), reads=['sVa%d' % tb])
                S.dma('pool', Vc.rearrange("(t j p) c -> t p j c", j=4, p=128)[t], sVc[tb][:].rearrange("p j h e -> p j (h e)"), reads=['sVc%d' % tb])
                S.op('act', lambda e: e.activation(out=cqs[:], in_=cq[:], func=AF.Square), reads=['cq'], writes=['cqs'])
                for c in range(3):
                    S.op('pe', lambda e: e.matmul(P[0][:], lhsT=ones_bf[:], rhs=cqs[:, c, :], start=(c == 0), stop=(c == 2)),
                         reads=['ones_bf', 'cqs'], writes=['P0'])
                rstd_from(rqbc[:], P[0][:], 1.0 / 384, ['P0'], ['rqbc'])
                for c in range(3):
                    S.op('dve', lambda e: e.tensor_scalar(out=cqg[:, c, :], in0=cq[:, c, :], scalar1=gains[:, gb + 24 + c:gb + 25 + c],
                                                          scalar2=None, op0=ALU.mult), reads=['cq', 'gains'], writes=['cqg'])
                for h in range(6):
                    b1 = frot.next()
                    for c in range(3):
                        S.op('pe', lambda e: e.matmul(P[b1][0:96, :], lhsT=wuq[:, c, h * 192:h * 192 + 96], rhs=cqg[:, c, :],
                                                      start=(c == 0), stop=(c == 2)), reads=['wuq', 'cqg'], writes=[PN[b1]])
                    for c in range(3):
                        S.op('pe', lambda e: e.matmul(P[7][0:96, :], lhsT=wuq[:, c, h * 192 + 96:h * 192 + 192], rhs=cqg[:, c, :],
                                                      start=(c == 0), stop=(c == 2)), reads=['wuq', 'cqg'], writes=['P7'])
                    S.op('dve', lambda e: e.tensor_tensor(out=sQb[tb][0:64, h, :], in0=P[b1][0:64, :], in1=rqbc[0:64, :], op=ALU.mult),
                         reads=[PN[b1], 'rqbc'], writes=['sQb%d' % tb])
                    S.op('dve', lambda e: e.tensor_tensor(out=t1[r, :], in0=P[b1][r, :], in1=cs_[r, 0, :], op=ALU.mult),
                         reads=[PN[b1], cn], writes=['t1'])
                    S.op('dve', lambda e: e.tensor_tensor(out=t2[r, :], in0=P[7][r, :], in1=cs_[r, 1, :], op=ALU.mult),
                         reads=['P7', cn], writes=['t2'])
                    S.op('dve', lambda e: e.tensor_tensor(out=t1[r, :], in0=t1[r, :], in1=t2[r, :], op=ALU.add),
                         reads=['t1', 't2'], writes=['t1'])
                    S.op('dve', lambda e: e.tensor_tensor(out=sQb[tb][r, h, :], in0=t1[r, :], in1=rqbc[r, :], op=ALU.mult),
                         reads=['t1', 'rqbc'], writes=['sQb%d' % tb])
                S.dma('pool', QbT[:, :, ts].rearrange("h p s -> p h s"), sQb[tb][:], reads=['sQb%d' % tb])
                S.op('act', lambda e: e.activation(out=ckvs[:], in_=ckv[:], func=AF.Square), reads=['ckv'], writes=['ckvs'])
                S.op('pe', lambda e: e.matmul(P[0][:], lhsT=ones_bf[:], rhs=ckvs[:], start=True, stop=True),
                     reads=['ones_bf', 'ckvs'], writes=['P0'])
                rstd_from(rkbc[:], P[0][:], 1.0 / 128, ['P0'], ['rkbc'])
                for j in range(4):
                    S.op('pe', lambda e: e.matmul(P[1][:, j:j + 1], lhsT=ckvs[:, j * 128:(j + 1) * 128], rhs=ones_bf[:, 0:1],
                                                  start=True, stop=True), reads=['ones_bf', 'ckvs'], writes=['P1'])
                rstd_from(rkcol[:], P[1][:, 0:4], 1.0 / 128, ['P1'], ['rkcol'])
                S.op('dve', lambda e: e.tensor_scalar(out=ckvg[:], in0=ckv[:], scalar1=gains[:, gb + 27:gb + 28], scalar2=None,
                                                      op0=ALU.mult), reads=['ckv', 'gains'], writes=['ckvg'])
                for hp in range(3):
                    b = frot.next()
                    S.op('pe', lambda e: e.matmul(P[b][:], lhsT=wukv[:, hp * 128:(hp + 1) * 128], rhs=ckvg[:], start=True, stop=True),
                         reads=['wukv', 'ckvg'], writes=[PN[b]])
                    evac_bc(sKb[tb][:, hp, :], b, rkbc, 'rkbc', ['sKb%d' % tb])
                kbv = KbT[:, 0:64, ts].rearrange("(hp two) p s -> two p hp s", two=2)
                S.dma('pool', kbv[0], sKb[tb][0:64, :, :], reads=['sKb%d' % tb])
                S.dma('pool', kbv[1], sKb[tb][64:128, :, :], reads=['sKb%d' % tb])
                for j in range(4):
                    js = slice(j * 128, (j + 1) * 128)
                    S.op('pe', lambda e: e.matmul(P[5][:, 0:384], lhsT=ckvg[:, js], rhs=wukv[:, 384:768], start=True, stop=True),
                         reads=['ckvg', 'wukv'], writes=['P5'])
                    S.op('act', lambda e: e.activation(out=sVb[tb][:, j, :, 0:64], in_=P[5][:, 0:384].rearrange("p (h e) -> p h e", e=64),
                                                       func=AF.Copy, scale=rkcol[:, j:j + 1]),
                         reads=['P5', 'rkcol'], writes=['sVb%d' % tb])
                S.dma('pool', Vb.rearrange("(t j p) c -> t p j c", j=4, p=128)[t], sVb[tb][:].rearrange("p j h e -> p j (h e)"), reads=['sVb%d' % tb])
            S.op('dve', lambda e: e.tensor_scalar(out=kmT[:], in0=kmacc[:], scalar1=1.0 / 256, scalar2=None, op0=ALU.mult),
                 reads=['kmacc'], writes=['kmT'])
            S.barrier()

    def finalize_head(oT_sb, oT_name, row0, es_sb, tag):
        zr, osg = es_sb
        for t in range(NT):
            ts = slice(t * TW, (t + 1) * TW)
            z_ = zr[t % 2]
            zn = tag + 'zr%d' % (t % 2)
            S.op('pe', lambda e: e.matmul(P[7][0:64, :], lhsT=sel65[0:65, :], rhs=oT_sb[0:65, ts], start=True, stop=True),
                 reads=['sel65', oT_name], writes=['P7'])
            S.op('act', lambda e: e.activation(out=z_[0:64, :], in_=P[7][0:64, :], func=AF.Ln), reads=['P7'], writes=[zn])
            S.op('act', lambda e: e.activation(out=z_[0:64, :], in_=z_[0:64, :], func=AF.Exp, scale=-1.0), reads=[zn], writes=[zn])
            o_ = osg[t % 2]
            S.op('dve', lambda e: e.tensor_tensor(out=o_[0:64, :], in0=oT_sb[0:64, ts], in1=z_[0:64, :], op=ALU.mult),
                 reads=[oT_name, zn], writes=[tag + 'osg%d' % (t % 2)])
            S.dma('pool', MIXT[row0:row0 + 64, ts], o_[0:64, :], reads=[tag + 'osg%d' % (t % 2)])

    LA = 2

    def phase_A(l):
        with ExitStack() as es:
            def sb(name, shape, dt):
                return es.enter_context(nc.sbuf_tensor(_uid("pa_" + name), list(shape), dt))
            qd = [[sb("q%d_%d" % (p, i), [128, S_LEN], BF16) for p in range(3)] for i in range(2)]
            kd = [[sb("k%d_%d" % (p, i), [128, S_LEN], BF16) for p in range(3)] for i in range(2)]
            vv = [[sb("v%d_%d" % (p, i), [128, 32, 65], BF16) for p in range(3)] for i in range(2)]
            dex = [sb("dex%d" % i, [128, 3, 256], F32) for i in range(2)]
            oT = [sb("oT%d" % i, [65, S_LEN], F32) for i in range(2)]
            E = [sb("E%d" % i, [128, 256], BF16) for i in range(4)]
            Pm = [sb("Pm%d" % i, [128, 256], BF16) for i in range(4)]
            zr = [sb("zr%d" % i, [64, TW], F32) for i in range(2)]
            osg = [sb("osg%d" % i, [64, TW], F32) for i in range(2)]
            for i in range(2):
                S.op('pool', lambda e: e.memset(qd[i][0][64:128, :], 0.0), writes=['pa_q0_%d' % i])
                S.op('pool', lambda e: e.memset(kd[i][0][64:128, :], 0.0), writes=['pa_k0_%d' % i])
            tiles = []
            for p in range(3):
                dil = PATTERNS[p][1]
                nA = S_LEN // dil // 128
                for kt in range(32):
                    a = kt % nA
                    nq = 256 if a + 1 < nA else 128
                    tiles.append((p, dil, kt // nA, a, kt, nq))

            def loads(h):
                hb = h % 2
                S.dma('sp', qd[hb][0][0:64, :], QaT[h * 64:(h + 1) * 64, :], writes=['pa_q0_%d' % hb])
                S.dma('sp', kd[hb][0][0:64, :], KaT[h * 64:(h + 1) * 64, :], writes=['pa_k0_%d' % hb])
                S.dma('sp', dex[hb][:], dexp_s[h].rearrange("p (a u) -> p a u", a=3), writes=['pa_dex%d' % hb])
                S.dma('sp', vv[hb][0][:], Va.rearrange("(kt p) (h e) -> p kt h e", p=128, e=65)[:, :, h, :], writes=['pa_v0_%d_0' % hb])
                for p in (1, 2):
                    dil = PATTERNS[p][1]
                    nA = S_LEN // dil // 128
                    vsrc = Va.rearrange("(a j r) (h e) -> j r a h e", j=128, r=dil, e=65)
                    for r in range(dil):
                        S.dma('sp', vv[hb][p][:, r * nA:(r + 1) * nA, :], vsrc[:, r, :, h, :], writes=['pa_v%d_%d_%d' % (p, hb, r)])
                    S.op('pool', lambda e: e.tensor_copy(out=qd[hb][p][:].rearrange("p (r m) -> p r m", r=dil),
                                                         in_=qd[hb][0][:].rearrange("p (m r) -> p r m", r=dil)),
                         reads=['pa_q0_%d' % hb], writes=['pa_q%d_%d' % (p, hb)])
                    S.op('pool', lambda e: e.tensor_copy(out=kd[hb][p][:].rearrange("p (r m) -> p r m", r=dil),
                                                         in_=kd[hb][0][:].rearrange("p (m r) -> p r m", r=dil)),
                         reads=['pa_k0_%d' % hb], writes=['pa_k%d_%d' % (p, hb)])

            srot = Rot([0, 1, 2, 3])
            orot = Rot([4, 5, 6])
            loads(0)
            S.op('pool', lambda e: e.memset(oT[0][:], 0.0), writes=['pa_oT0'])
            for h in range(H_DIL):
                hb = h % 2
                if h + 1 < H_DIL:
                    loads(h + 1)

                def qk(i):
                    p, dil, r, a, kt, nq = tiles[i]
                    b = srot.next()
                    S.op('pe', lambda e: e.matmul(P[b][:, 0:nq], lhsT=kd[hb][p][:, kt * 128:(kt + 1) * 128],
                                                  rhs=qd[hb][p][:, kt * 128:kt * 128 + nq], start=True, stop=True),
                         reads=['pa_k%d_%d' % (p, hb), 'pa_q%d_%d' % (p, hb)], writes=[PN[b]])
                    ei = i % 4
                    S.op('act', lambda e: e.activation(out=E[ei][:, 0:nq], in_=P[b][:, 0:nq], func=AF.Exp, scale=0.125),
                         reads=[PN[b]], writes=['pa_E%d' % ei])
                    S.op('dve', lambda e: e.tensor_tensor(out=Pm[ei][:, 0:nq], in0=E[ei][:, 0:nq], in1=dex[hb][:, p, 0:nq], op=ALU.mult),
                         reads=['pa_E%d' % ei, 'pa_dex%d' % hb], writes=['pa_Pm%d' % ei])

                def pv(i):
                    p, dil, r, a, kt, nq = tiles[i]
                    ei = i % 4
                    b = orot.next()
                    S.op('pe', lambda e: e.matmul(P[b][0:65, 0:nq], lhsT=vv[hb][p][:, kt, :], rhs=Pm[ei][:, 0:nq], start=True, stop=True),
                         reads=['pa_v%d_%d_%d' % (p, hb, r), 'pa_Pm%d' % ei], writes=[PN[b]])
                    ov = oT[hb][:].rearrange("p (m r) -> p r m", r=dil)[:, r, a * 128:a * 128 + nq]
                    S.op('dve', lambda e: e.tensor_tensor(out=ov, in0=P[b][0:65, 0:nq], in1=ov, op=ALU.add),
                         reads=[PN[b], 'pa_oT%d' % hb], writes=['pa_oT%d' % hb])

                n = len(tiles)
                for i in range(n + LA):
                    if i < n:
                        qk(i)
                    if i >= LA:
                        pv(i - LA)
                    if i == 10:
                        if h > 0:
                            finalize_head(oT[1 - hb], 'pa_oT%d' % (1 - hb), (h - 1) * 64, (zr, osg), 'pa_')
                        if h + 1 < H_DIL:
                            S.op('pool', lambda e: e.memset(oT[1 - hb][:], 0.0), writes=['pa_oT%d' % (1 - hb)])
            finalize_head(oT[(H_DIL - 1) % 2], 'pa_oT%d' % ((H_DIL - 1) % 2), (H_DIL - 1) * 64, (zr, osg), 'pa_')
            S.barrier()

    def causal_phase(l, kind):
        moba = kind == 'c'
        nh = H_MOBA if moba else H_MLA
        dq = 128 if moba else 96
        scale = 0.125 if moba else 96 ** -0.5
        with ExitStack() as es:
            def sb(name, shape, dt):
                return es.enter_context(nc.sbuf_tensor(_uid("pc_" + name), list(shape), dt))
            qT = [sb("qT%d" % i, [dq, S_LEN], BF16) for i in range(2)]
            kT = [sb("kT%d" % i, [dq, S_LEN], BF16) for i in range(2)]
            v = [sb("v%d" % i, [128, 32, 65], BF16) for i in range(2)]
            E = [sb("E%d" % i, [128, TW], BF16) for i in range(4)]
            oS = [sb("oS%d" % i, [65, S_LEN], F32) for i in range(2)]
            zr = [sb("zr%d" % i, [64, TW], F32) for i in range(2)]
            osg = [sb("osg%d" % i, [64, TW], F32) for i in range(2)]
            if moba:
                mex = [sb("mex%d" % i, [128, MSTRIP_W], F32) for i in range(2)]
                Pm = [sb("Pm%d" % i, [128, TW], BF16) for i in range(4)]
                negT = [sb("negT%d" % i, [16, S_LEN], BF16) for i in range(2)]
                kmh = [sb("kmh%d" % i, [128, 16], BF16) for i in range(2)]
                gm = [sb("gm%d" % i, [128, 16], F32) for i in range(2)]
                top8 = [sb("top8%d" % i, [128, 8], F32) for i in range(2)]
                selq = [sb("selq%d" % i, [128, 16], F32) for i in range(2)]
                for i in range(2):
                    S.op('pool', lambda e: e.memset(qT[i][64:128, :], 0.0), writes=['pc_qT%d' % i])
                    S.op('pool', lambda e: e.memset(kT[i][64:128, :], 0.0), writes=['pc_kT%d' % i])
                    S.op('pool', lambda e: e.memset(kmh[i][64:128, :], 0.0), writes=['pc_kmh%d' % i])

            def loads(h):
                hb = h % 2
                if moba:
                    S.dma('sp', qT[hb][0:64, :], QcT[h * 64:(h + 1) * 64, :], writes=['pc_qT%d' % hb])
                    S.dma('sp', kT[hb][0:64, :], KcT[h * 64:(h + 1) * 64, :], writes=['pc_kT%d' % hb])
                    S.dma('sp', v[hb][:], Vc.rearrange("(kt p) (h e) -> p kt h e", p=128, e=65)[:, :, h, :], writes=['pc_v%d' % hb])
                    S.dma('sp', mex[hb][:], mexp_s[h], writes=['pc_mex%d' % hb])
                    S.dma('sp', kmh[hb][0:64, :], kmT[(h % 2) * 64:(h % 2) * 64 + 64, h // 2, :], reads=['kmT'], writes=['pc_kmh%d' % hb])
                else:
                    S.dma('sp', qT[hb][:], QbT[h], writes=['pc_qT%d' % hb])
                    S.dma('sp', kT[hb][:], KbT[h], writes=['pc_kT%d' % hb])
                    S.dma('sp', v[hb][:], Vb.rearrange("(kt p) (h e) -> p kt h e", p=128, e=65)[:, :, h, :], writes=['pc_v%d' % hb])

            def prep(h):
                hb = h % 2
                for i in range(32):
                    j = i // 2
                    ib = i % 2
                    g_, t8, sq_ = gm[ib], top8[ib], selq[ib]
                    gn, tn, sn = 'pc_gm%d' % ib, 'pc_top8%d' % ib, 'pc_selq%d' % ib
                    S.op('pe', lambda e: e.matmul(P[6][:, ib * 16:ib * 16 + 16], lhsT=qT[hb][:, i * 128:(i + 1) * 128], rhs=kmh[hb][:],
                                                  start=True, stop=True), reads=['pc_qT%d' % hb, 'pc_kmh%d' % hb], writes=['P6_%d' % ib])
                    S.op('dve', lambda e: e.tensor_tensor(out=g_[:], in0=P[6][:, ib * 16:ib * 16 + 16], in1=mtab[:, j * 16:(j + 1) * 16], op=ALU.add),
                         reads=['P6_%d' % ib, 'mtab'], writes=[gn])
                    S.op('dve', lambda e: e.max(out=t8[:], in_=g_[:]), reads=[gn], writes=[tn])
                    S.op('dve', lambda e: e.tensor_scalar(out=sq_[:], in0=g_[:], scalar1=t8[:, 2:3], scalar2=None, op0=ALU.is_ge),
                         reads=[gn, tn], writes=[sn])
                    S.op('dve', lambda e: e.tensor_tensor(out=sq_[:], in0=sq_[:], in1=mtab[:, 256 + j * 16:256 + (j + 1) * 16], op=ALU.mult),
                         reads=[sn, 'mtab'], writes=[sn])
                    S.op('dve', lambda e: e.tensor_tensor(out=sq_[:], in0=sq_[:], in1=mtab[:, 512 + j * 16:512 + (j + 1) * 16], op=ALU.add),
                         reads=[sn, 'mtab'], writes=[sn])
                    S.op('dve', lambda e: e.tensor_scalar(out=sq_[:], in0=sq_[:], scalar1=-1.0, scalar2=-NEG, op0=ALU.add, op1=ALU.mult),
                         reads=[sn], writes=[sn])
                    S.op('pe', lambda e: e.transpose(out=P[6][0:16, 128 + ib * 128:256 + ib * 128], in_=sq_[:], identity=ident[:]),
                         reads=[sn, 'ident'], writes=['P6t_%d' % ib])
                    S.op('act', lambda e: e.activation(out=negT[hb][:, i * 128:(i + 1) * 128], in_=P[6][0:16, 128 + ib * 128:256 + ib * 128], func=AF.Copy),
                         reads=['P6t_%d' % ib], writes=['pc_negT%d' % hb])

            tiles = []
            for qg in range(NT):
                nk = 4 * qg + 4
                for kt in range(nk):
                    c = max(0, kt - 4 * qg)
                    tiles.append((qg, kt, c, kt == 0, kt == nk - 1))
            srot = Rot([0, 1, 2, 3])
            orot = Rot([4, 5])
            ob = [None]
            loads(0)
            if moba:
                prep(0)
            for h in range(nh):
                hb = h % 2
                if h + 1 < nh:
                    loads(h + 1)

                def qk(i):
                    qg, kt, c, first, last = tiles[i]
                    c0 = c * 128
                    nq = TW - c0
                    q0 = qg * TW + c0
                    b = srot.next()
                    S.op('pe', lambda e: e.matmul(P[b][:, 0:nq], lhsT=kT[hb][:, kt * 128:(kt + 1) * 128], rhs=qT[hb][:, q0:q0 + nq],
                                                  start=True, stop=not moba), reads=['pc_kT%d' % hb, 'pc_qT%d' % hb], writes=[PN[b]])
                    if moba:
                        n = kt // 2
                        S.op('pe', lambda e: e.matmul(P[b][:, 0:nq], lhsT=e16[:, n * 128:(n + 1) * 128], rhs=negT[hb][:, q0:q0 + nq],
                                                      start=False, stop=True), reads=['e16', 'pc_negT%d' % hb], writes=[PN[b]])
                    ei = i % 4
                    S.op('act', lambda e: e.activation(out=E[ei][:, 0:nq], in_=P[b][:, 0:nq], func=AF.Exp, scale=scale),
                         reads=[PN[b]], writes=['pc_E%d' % ei])
                    if moba:
                        u0 = min(q0 - kt * 128 + 384, MSTRIP_CLAMP)
                        S.op('dve', lambda e: e.tensor_tensor(out=Pm[ei][:, 0:nq], in0=E[ei][:, 0:nq], in1=mex[hb][:, u0:u0 + nq], op=ALU.mult),
                             reads=['pc_E%d' % ei, 'pc_mex%d' % hb], writes=['pc_Pm%d' % ei])
                    elif kt >= 4 * qg:
                        S.op('dve', lambda e: e.tensor_tensor(out=E[ei][:, 0:128], in0=E[ei][:, 0:128], in1=tri[:], op=ALU.mult),
                             reads=['pc_E%d' % ei, 'tri'], writes=['pc_E%d' % ei])

                def pv(i):
                    qg, kt, c, first, last = tiles[i]
                    c0 = c * 128
                    nq = TW - c0
                    ei = i % 4
                    if first:
                        ob[0] = orot.next()
                    b = ob[0]
                    src_, sn = (Pm[ei], 'pc_Pm%d' % ei) if moba else (E[ei], 'pc_E%d' % ei)
                    S.op('pe', lambda e: e.matmul(P[b][0:65, c0:TW], lhsT=v[hb][:, kt, :], rhs=src_[:, 0:nq], start=first, stop=last),
                         reads=['pc_v%d' % hb, sn], writes=[PN[b]])
                    if last:
                        S.op('act', lambda e: e.activation(out=oS[hb][:, qg * TW:(qg + 1) * TW], in_=P[b][0:65, :], func=AF.Copy),
                             reads=[PN[b]], writes=['pc_oS%d' % hb])

                n = len(tiles)
                for i in range(n + LA):
                    if i < n:
                        qk(i)
                    if i >= LA:
                        pv(i - LA)
                    if i == 10 and h > 0:
                        row0 = (768 if moba else 384) + (h - 1) * 64
                        finalize_head(oS[1 - hb], 'pc_oS%d' % (1 - hb), row0, (zr, osg), 'pc_')
                    if i == 60 and moba and h + 1 < nh:
                        prep(h + 1)
            row0 = (768 if moba else 384) + (nh - 1) * 64
            finalize_head(oS[(nh - 1) % 2], 'pc_oS%d' % ((nh - 1) % 2), row0, (zr, osg), 'pc_')
            S.barrier()

    def phase_O(l, last_layer):
        gb = l * GPL
        src = xT_in if l == 0 else XT
        srcv = src.rearrange("(c p) s -> p c s", p=128)
        dstv = XT.rearrange("(c p) s -> p c s", p=128)
        mixv = MIXT.rearrange("(c p) s -> p c s", p=128)
        yv = yT.rearrange("(c p) s -> p c s", p=128)
        with ExitStack() as es:
            def sb(name, shape, dt):
                return es.enter_context(nc.sbuf_tensor(_uid("po_" + name), list(shape), dt))
            wo = sb("wo", [128, 8, 1024], BF16)
            wup = [sb("wup%d" % i, [128, 4, 8, 128], BF16) for i in range(2)]
            wdn = [sb("wdn%d" % i, [128, 32, 128], BF16) for i in range(2)]
            xt = sb("xt", [128, 8, TW], F32)
            mx = sb("mx", [128, 8, TW], F32)
            sq = sb("sq", [128, 8, TW], BF16)
            mn = sb("mn", [128, 8, TW], BF16)
            rg = [sb("rg%d" % i, [128, TW], F32) for i in range(3)]
            x1 = sb("x1", [128, 8, TW], F32)
            hg = sb("hg", [128, 8, TW], BF16)
            rbc = sb("rbc", [128, TW], F32)
            r2 = sb("r2", [128, TW], F32)
            rr = sb("rr", [128, TW], BF16)
            u = sb("u", [128, 32, TW], BF16)
            yo = sb("yo", [128, 8, TW], F32)
            S.dma('sp', wo[:], wo_b[l].rearrange("p (c n) -> p c n", c=8), reads=wres('wo', l, 8192), writes=['po_wo'])
            prot = Rot([1, 2, 3, 4, 5, 6, 7])
            groups = ((0, 3), (3, 6), (6, 8))
            for t in range(NT):
                ts = slice(t * TW, (t + 1) * TW)
                S.dma('sp', xt[:], srcv[:, :, ts], writes=['po_xt'])
                S.dma('sp', mx[:], mixv[:, :, ts], writes=['po_mx'])
                S.op('act', lambda e: e.activation(out=sq[:], in_=mx[:], func=AF.Square), reads=['po_mx'], writes=['po_sq'])
                for gi, (c0, c1) in enumerate(groups):
                    for c in range(c0, c1):
                        S.op('pe', lambda e: e.matmul(P[0][:], lhsT=ones_bf[:], rhs=sq[:, c, :], start=(c == c0), stop=(c == c1 - 1)),
                             reads=['ones_bf', 'po_sq'], writes=['P0'])
                    rstd_from(rg[gi][:], P[0][:], 1.0 / ((c1 - c0) * 128), ['P0'], ['po_rg%d' % gi])
                    for c in range(c0, c1):
                        S.op('dve', lambda e: e.scalar_tensor_tensor(out=mn[:, c, :], in0=mx[:, c, :], scalar=gains[:, gb + 16 + c:gb + 17 + c],
                                                                     in1=rg[gi][:], op0=ALU.mult, op1=ALU.mult),
                             reads=['po_mx', 'gains', 'po_rg%d' % gi], writes=['po_mn'])
                for oc in range(8):
                    b = prot.next()
                    for c in range(8):
                        S.op('pe', lambda e: e.matmul(P[b][:], lhsT=wo[:, c, oc * 128:(oc + 1) * 128], rhs=mn[:, c, :],
                                                      start=(c == 0), stop=(c == 7)), reads=['po_wo', 'po_mn'], writes=[PN[b]])
                    S.op('dve', lambda e: e.tensor_tensor(out=x1[:, oc, :], in0=P[b][:], in1=xt[:, oc, :], op=ALU.add),
                         reads=[PN[b], 'po_xt'], writes=['po_x1'])
                S.op('act', lambda e: e.activation(out=sq[:], in_=x1[:], func=AF.Square), reads=['po_x1'], writes=['po_sq'])
                for c in range(8):
                    S.op('pe', lambda e: e.matmul(P[0][:], lhsT=ones_bf[:], rhs=sq[:, c, :], start=(c == 0), stop=(c == 7)),
                         reads=['ones_bf', 'po_sq'], writes=['P0'])
                rstd_from(rbc[:], P[0][:], 1.0 / D, ['P0'], ['po_rbc'])
                S.op('pool', lambda e: e.tensor_tensor(out=r2[:], in0=rbc[:], in1=rbc[:], op=ALU.mult), reads=['po_rbc'], writes=['po_r2'])
                for c in range(8):
                    S.op('dve', lambda e: e.tensor_scalar(out=hg[:, c, :], in0=x1[:, c, :], scalar1=gains[:, gb + 8 + c:gb + 9 + c],
                                                          scalar2=None, op0=ALU.mult), reads=['po_x1', 'gains'], writes=['po_hg'])
                for jg in range(8):
                    wb_ = wup[jg % 2]
                    wn = 'po_wup%d' % (jg % 2)
                    S.dma('sp', wb_[:], wup_b[l, jg * 4:(jg + 1) * 4].rearrange("j p (c m) -> p j c m", c=8),
                          reads=['wup_b%d_%d' % (l, jg * 4 + q) for q in range(4)], writes=[wn])
                    for jj in range(4):
                        j = jg * 4 + jj
                        b = prot.next()
                        for c in range(8):
                            S.op('pe', lambda e: e.matmul(P[b][:], lhsT=wb_[:, jj, c, :], rhs=hg[:, c, :], start=(c == 0), stop=(c == 7)),
                                 reads=[wn, 'po_hg'], writes=[PN[b]])
                        S.op('act', lambda e: e.activation(out=rr[:], in_=P[b][:], func=AF.Relu), reads=[PN[b]], writes=['po_rr'])
                        S.op('dve', lambda e: e.tensor_tensor(out=u[:, j, :], in0=rr[:], in1=rr[:], op=ALU.mult),
                             reads=['po_rr'], writes=['po_u'])
                for oc in range(8):
                    wb_ = wdn[oc % 2]
                    wn = 'po_wdn%d' % (oc % 2)
                    S.dma('sp', wb_[:], wdn_b[l, oc].rearrange("p (k m) -> p k m", k=32), reads=['wdn_b%d_%d' % (l, oc)], writes=[wn])
                    b = prot.next()
                    for k in range(32):
                        S.op('pe', lambda e: e.matmul(P[b][:], lhsT=wb_[:, k, :], rhs=u[:, k, :], start=(k == 0), stop=(k == 31)),
                             reads=[wn, 'po_u'], writes=[PN[b]])
                    S.op('dve', lambda e: e.tensor_tensor(out=yo[:, oc, :], in0=P[b][:], in1=r2[:], op=ALU.mult),
                         reads=[PN[b], 'po_r2'], writes=['po_yo'])
                    S.op('pool', lambda e: e.tensor_tensor(out=yo[:, oc, :], in0=yo[:, oc, :], in1=x1[:, oc, :], op=ALU.add),
                         reads=['po_yo', 'po_x1'], writes=['po_yo'])
                if not last_layer:
                    S.dma('pool', dstv[:, :, ts], yo[:], reads=['po_yo'])
                elif not final_norm:
                    S.dma('pool', yv[:, :, ts], yo[:], reads=['po_yo'])
                else:
                    gf = GPL * L
                    S.op('act', lambda e: e.activation(out=sq[:], in_=yo[:], func=AF.Square), reads=['po_yo'], writes=['po_sq'])
                    for c in range(8):
                        S.op('pe', lambda e: e.matmul(P[0][:], lhsT=ones_bf[:], rhs=sq[:, c, :], start=(c == 0), stop=(c == 7)),
                             reads=['ones_bf', 'po_sq'], writes=['P0'])
                    rstd_from(rbc[:], P[0][:], 1.0 / D, ['P0'], ['po_rbc'])
                    for c in range(8):
                        S.op('dve', lambda e: e.scalar_tensor_tensor(out=x1[:, c, :], in0=yo[:, c, :], scalar=gains[:, gf + c:gf + c + 1],
                                                                     in1=rbc[:], op0=ALU.mult, op1=ALU.mult),
                             reads=['po_yo', 'gains', 'po_rbc'], writes=['po_x1'])
                    S.dma('pool', yv[:, :, ts], x1[:], reads=['po_x1'])
            S.barrier()

    for l in range(L):
        if 'P' in phases:
            phase_P(l)
        if l + 1 < L:
            cast_weights(l + 1)
        if 'A' in phases:
            phase_A(l)
        if 'B' in phases:
            causal_phase(l, 'b')
        if 'C' in phases:
            causal_phase(l, 'c')
        if debug and l == 0:
            S.dma('sp', dbg['mix'], MIXT)
            S.barrier()
        if 'O' in phases:
            phase_O(l, l == L - 1)
    S.barrier()
    return nc, S


_HOST_INPUT_NAMES = ("w_in", "w_uq", "w_ukv", "w_o", "w_up", "w_down", "gains",
                     "dstrip", "mstrip", "rope", "ident", "sel65", "e16", "tri", "mtab")


def kernel(x, g_attn, w_in, g_q_lora, g_kv_lora, w_uq, w_ukv, rel_bias, g_mix, w_o, g_mlp, w_up, w_down, g_final,
           _depth=DEPTH, _debug=False):
    inp = dict(g_attn=np.asarray(g_attn), w_in=np.asarray(w_in), g_q_lora=np.asarray(g_q_lora), g_kv_lora=np.asarray(g_kv_lora),
               w_uq=np.asarray(w_uq), w_ukv=np.asarray(w_ukv), g_mix=np.asarray(g_mix), w_o=np.asarray(w_o),
               g_mlp=np.asarray(g_mlp), w_up=np.asarray(w_up), w_down=np.asarray(w_down), g_final=np.asarray(g_final))
    if _depth != DEPTH:
        for k in list(inp):
            if k != 'g_final':
                inp[k] = inp[k][:_depth]
    x = np.asarray(x)
    rel_bias = np.asarray(rel_bias, dtype=np.float32)
    if LAUNCH_MODE == 'multi' and not _debug:
        consts = _host_consts(rel_bias)
        progs = {}
        cur = [np.ascontiguousarray(x[c].T) for c in range(NCORES)]
        for l in range(_depth):
            last = l == _depth - 1
            if last not in progs:
                _UID[0] = 0
                progs[last] = build_program(1, False, 'PABCO', final_norm=last)[0]
            li = {k: (v if k == 'g_final' else v[l:l + 1]) for k, v in inp.items()}
            shared = _host_weights(li)
            shared.update(consts)
            in_maps = []
            for c in range(NCORES):
                m = {k: shared[k] for k in _HOST_INPUT_NAMES}
                m["xT"] = cur[c]
                in_maps.append(m)
            res = run_bass_kernel_spmd(progs[last], in_maps, core_ids=list(range(NCORES)))
            cur = [np.ascontiguousarray(res.results[c]["yT"]) for c in range(NCORES)]
        return np.stack([np.ascontiguousarray(cur[c].T) for c in range(NCORES)], 0).astype(np.float32)
    shared = _host_weights(inp)
    shared.update(_host_consts(rel_bias))
    nc, _ = build_program(_depth, _debug)
    in_maps = []
    for c in range(NCORES):
        m = {k: shared[k] for k in _HOST_INPUT_NAMES}
        m["xT"] = np.ascontiguousarray(x[c].T)
        in_maps.append(m)
    res = run_bass_kernel_spmd(nc, in_maps, core_ids=list(range(NCORES)))
    out = np.stack([np.ascontiguousarray(res.results[c]["yT"].T) for c in range(NCORES)], 0).astype(np.float32)
    if _debug:
        return out, res
    return out
```

```python
import math
from contextlib import ExitStack

import numpy as np
import concourse.bass as bass
import concourse.mybir as mybir
from concourse.bass_utils import run_bass_kernel_spmd

F32 = mybir.dt.float32
BF16 = mybir.dt.bfloat16
AF = mybir.ActivationFunctionType
ALU = mybir.AluOpType
AX = mybir.AxisListType

D = 1024
S_LEN = 4096
DEPTH = 4
NCORES = 8
TW = 512
NT = S_LEN // TW
EPS = 1e-6
NEG = -30000.0
H_DIL, H_MLA, H_MOBA = 6, 6, 4
PATTERNS = ((128, 1), (512, 4), (2048, 16))
WIN_COLS = 2048 + 640
MSTRIP_W = 2560
MSTRIP_CLAMP = 2048
GPL = 28
LAUNCH_MODE = 'fused'


class Sched:
    def __init__(self, nc, ndma=(('sp', 16), ('pool', 40))):
        self.nc = nc
        self.eng = {'pe': nc.tensor, 'act': nc.scalar, 'dve': nc.vector, 'pool': nc.gpsimd, 'sp': nc.sync}
        self.sem = {}
        self.val = {}
        self.kidx = {}
        for e in self.eng:
            self.sem[e] = nc.alloc_semaphore('s_' + e)
            self.val[e] = 0
            self.kidx[e] = len(self.kidx)
        self.dq = {}
        for q, n in ndma:
            lst = []
            for i in range(n):
                k = 'd_%s_%d' % (q, i)
                self.sem[k] = nc.alloc_semaphore(k)
                self.val[k] = 0
                self.kidx[k] = len(self.kidx)
                lst.append(k)
            self.dq[q] = [lst, 0]
        nk = len(self.kidx)
        self.vc = {e: np.zeros(nk, np.int64) for e in self.eng}
        self.snap = {}
        self.res = {}
        self.n_ins = 0
        self.n_wait = {e: 0 for e in self.eng}

    def _wait(self, e, key, v):
        ki = self.kidx[key]
        if v <= 0 or self.vc[e][ki] >= v:
            return
        self.eng[e].wait_ge(self.sem[key], v)
        self.n_wait[e] += 1
        sn = self.snap.get((key, v))
        if sn is not None and key != e:
            np.maximum(self.vc[e], sn, out=self.vc[e])
        self.vc[e][ki] = v

    def _deps(self, e, reads, writes, own=None):
        need = {}
        for r in reads:
            st = self.res.get(r)
            if st and st['w']:
                k, v = st['w']
                if k == own and e == 'pe':
                    continue
                need[k] = max(need.get(k, 0), v)
        for w in writes:
            st = self.res.get(w)
            if not st:
                continue
            if st['w'] and st['w'][0] != own:
                k, v = st['w']
                need[k] = max(need.get(k, 0), v)
            for k, v in st['r'].items():
                if k != own:
                    need[k] = max(need.get(k, 0), v)
        for k, v in sorted(need.items(), key=lambda kv: -kv[1]):
            self._wait(e, k, v)

    def _record(self, key, v, reads, writes):
        for r in reads:
            st = self.res.setdefault(r, {'w': None, 'r': {}})
            st['r'][key] = max(st['r'].get(key, 0), v)
        for w in writes:
            self.res[w] = {'w': (key, v), 'r': {}}

    def op(self, e, fn, reads=(), writes=()):
        self._deps(e, reads, writes, own=e)
        ins = fn(self.eng[e])
        self.val[e] += 1
        ins.then_inc(self.sem[e], 1)
        sn = self.vc[e].copy()
        self.snap[(e, self.val[e])] = sn
        self._record(e, self.val[e], reads, writes)
        self.n_ins += 1
        return ins

    def dma(self, q, out, in_, reads=(), writes=()):
        lst, idx = self.dq[q]
        key = lst[idx % len(lst)]
        self.dq[q][1] = idx + 1
        self._wait(q, key, self.val[key])
        self._deps(q, reads, writes)
        ins = self.eng[q].dma_start(out=out, in_=in_)
        self.val[key] += 16
        ins.then_inc(self.sem[key], 16)
        self.snap[(key, self.val[key])] = self.vc[q].copy()
        self._record(key, self.val[key], reads, writes)
        self.n_ins += 1
        return ins

    def barrier(self):
        for e in self.eng:
            for k in self.sem:
                self._wait(e, k, self.val[k])


_UID = [0]


def _uid(name):
    _UID[0] += 1
    return "%s_%d" % (name, _UID[0])


class Rot:
    def __init__(self, items):
        self.items = list(items)
        self.i = 0

    def next(self):
        it = self.items[self.i % len(self.items)]
        self.i += 1
        return it


def _t5_bucket(d):
    n = np.maximum(d, 0)
    nf = np.maximum(n, 1).astype(np.float32)
    large = 16 + (np.log(nf / np.float32(16)) / np.float32(math.log(2048 / 16)) * np.float32(16)).astype(np.int32)
    large = np.minimum(large, 31)
    return np.where(n < 16, n, large)


def _host_consts(rel_bias):
    c = {}
    j = np.arange(128)[:, None]
    u = np.arange(256)[None, :]
    dd = u - j
    valid = (dd >= 0) & (dd <= 128)
    ds = np.full((3, H_DIL, 128, 256), NEG, np.float32)
    for p, (_, dil) in enumerate(PATTERNS):
        bk = _t5_bucket(dd * dil)
        for h in range(H_DIL):
            ds[p, h] = np.where(valid, rel_bias[bk, h], np.float32(NEG))
    c['dstrip'] = np.ascontiguousarray(ds.transpose(1, 2, 0, 3).reshape(H_DIL, 128, 3 * 256))
    u = np.arange(MSTRIP_W)[None, :]
    d = u - 384 - j
    bk = _t5_bucket(d)
    ms = np.zeros((H_MOBA, 128, MSTRIP_W), np.float32)
    for h in range(H_MOBA):
        ms[h] = np.where(d >= 0, rel_bias[bk, H_DIL + h], np.float32(NEG))
    c['mstrip'] = ms
    inv_freq = (np.float32(10000.0) ** (-np.arange(0, 32, 2, dtype=np.float32) / np.float32(32))).astype(np.float32)
    ang = (np.arange(S_LEN, dtype=np.float32)[:, None] * inv_freq[None, :]).astype(np.float32)
    cs, sn = np.cos(ang).astype(np.float32).T, np.sin(ang).astype(np.float32).T
    c['rope'] = np.ascontiguousarray(np.stack([np.concatenate([cs, cs], 0), np.concatenate([-sn, sn], 0)], 0))
    c['ident'] = np.eye(128, dtype=np.float32)
    sel = np.zeros((128, 64), np.float32)
    sel[64, :] = 1.0
    c['sel65'] = sel
    c['blk1h'] = (np.arange(S_LEN)[None, :] // 256 == np.arange(16)[:, None]).astype(np.float32)
    tri = (np.arange(128)[None, :] >= np.arange(128)[:, None]).astype(np.float32)
    c['tri'] = tri
    jj = np.arange(16)[:, None]
    nn = np.arange(16)[None, :]
    vm = np.where(nn < jj, 0.0, -1e30).astype(np.float32)
    va = (nn < jj).astype(np.float32)
    ow = (nn == jj).astype(np.float32)
    c['mtab'] = np.ascontiguousarray(np.broadcast_to(np.stack([vm, va, ow], 0).reshape(1, 3 * 256), (128, 768)))
    return c


def _fm(g):
    return np.ascontiguousarray(g.reshape(-1, 128).T)


def _host_weights(inp):
    w = {}
    w_in = inp['w_in']
    Lr = w_in.shape[0]
    zeros64 = np.zeros((Lr, D, 64), np.float32)
    zeros32 = np.zeros((Lr, D, 32), np.float32)
    kr = w_in[:, :, 1664:1696]
    krs = np.concatenate([kr[:, :, 16:32], kr[:, :, 0:16]], -1)
    fm = np.concatenate([
        w_in[:, :, 0:384], w_in[:, :, 384:768], w_in[:, :, 1152:1536], w_in[:, :, 1536:1664],
        zeros64, kr, zeros32, zeros64, krs, zeros32,
        w_in[:, :, 1696:1952], w_in[:, :, 1952:2208],
        w_in[:, :, 768:1152], w_in[:, :, 2208:2464]], -1)
    assert fm.shape[-1] == WIN_COLS
    w['w_in'] = np.ascontiguousarray(fm.reshape(Lr, 8, 128, WIN_COLS).transpose(0, 2, 1, 3)).reshape(Lr, 128, 8 * WIN_COLS)
    uq = inp['w_uq'].reshape(Lr, 384, 6, 96)
    z = np.zeros((Lr, 384, 6, 64), np.float32)
    uqs = np.concatenate([z, uq[..., 80:96], uq[..., 64:80]], -1)
    uqr = np.stack([uq, uqs], 3).reshape(Lr, 384, 6 * 2 * 96)
    w['w_uq'] = np.ascontiguousarray(uqr.reshape(Lr, 3, 128, 1152).transpose(0, 2, 1, 3)).reshape(Lr, 128, 3 * 1152)
    ukv = inp['w_ukv'].reshape(Lr, 128, 6, 128)
    w['w_ukv'] = np.ascontiguousarray(np.concatenate([ukv[..., 0:64].reshape(Lr, 128, 384),
                                                      ukv[..., 64:128].reshape(Lr, 128, 384)], -1))
    w['w_o'] = np.ascontiguousarray(inp['w_o'].reshape(Lr, 8, 128, 1024).transpose(0, 2, 1, 3)).reshape(Lr, 128, 8192)
    w['w_up'] = np.ascontiguousarray(inp['w_up'].reshape(Lr, 8, 128, 32, 128).transpose(0, 3, 2, 1, 4)).reshape(Lr, 32, 128, 1024)
    w['w_down'] = np.ascontiguousarray(inp['w_down'].reshape(Lr, 32, 128, 8, 128).transpose(0, 3, 2, 1, 4)).reshape(Lr, 8, 128, 4096)
    g = np.zeros((128, GPL * Lr + 8), np.float32)
    for l in range(Lr):
        b = l * GPL
        g[:, b:b + 8] = _fm(inp['g_attn'][l])
        g[:, b + 8:b + 16] = _fm(inp['g_mlp'][l])
        g[:, b + 16:b + 24] = _fm(inp['g_mix'][l])
        g[:, b + 24:b + 27] = _fm(inp['g_q_lora'][l])
        g[:, b + 27:b + 28] = _fm(inp['g_kv_lora'][l])
    g[:, GPL * Lr:] = _fm(inp['g_final'])
    w['gains'] = g
    return w


def build_program(depth=DEPTH, debug=False, phases='PABCO', final_norm=True):
    nc = bass.Bass("TRN2", target_bir_lowering=False)
    S = Sched(nc)
    L = depth

    def din(name, shape, dt=F32):
        return nc.dram_tensor(name, list(shape), dt, kind="ExternalInput").ap()

    def dscr(name, shape, dt):
        return nc.dram_tensor(name, list(shape), dt).ap()

    xT_in = din("xT", [D, S_LEN])
    win_f = din("w_in", [L, 128, 8 * WIN_COLS])
    wuq_f = din("w_uq", [L, 128, 3 * 1152])
    wukv_f = din("w_ukv", [L, 128, 768])
    wo_f = din("w_o", [L, 128, 8192])
    wup_f = din("w_up", [L, 32, 128, 1024])
    wdn_f = din("w_down", [L, 8, 128, 4096])
    gains_d = din("gains", [128, GPL * L + 8])
    dstrip_d = din("dstrip", [H_DIL, 128, 768])
    mstrip_d = din("mstrip", [H_MOBA, 128, MSTRIP_W])
    rope_d = din("rope", [2, 32, S_LEN])
    ident_d = din("ident", [128, 128])
    sel65_d = din("sel65", [128, 64])
    blk1h_d = din("blk1h", [16, S_LEN])
    tri_d = din("tri", [128, 128])
    mtab_d = din("mtab", [128, 768])
    yT = nc.dram_tensor("yT", [D, S_LEN], F32, kind="ExternalOutput").ap()

    win_b = dscr("win_b", [L, 128, 8 * WIN_COLS], BF16)
    wuq_b = dscr("wuq_b", [L, 128, 3 * 1152], BF16)
    wukv_b = dscr("wukv_b", [L, 128, 768], BF16)
    wo_b = dscr("wo_b", [L, 128, 8192], BF16)
    wup_b = dscr("wup_b", [L, 32, 128, 1024], BF16)
    wdn_b = dscr("wdn_b", [L, 8, 128, 4096], BF16)
    XT = dscr("XT", [D, S_LEN], F32)
    QaT = dscr("QaT", [384, S_LEN], BF16)
    KaT = dscr("KaT", [384, S_LEN], BF16)
    Va = dscr("Va", [S_LEN, 6 * 64], BF16)
    QbT = dscr("QbT", [6, 96, S_LEN], BF16)
    KbT = dscr("KbT", [6, 96, S_LEN], BF16)
    Vb = dscr("Vb", [S_LEN, 6 * 64], BF16)
    QcT = dscr("QcT", [256, S_LEN], BF16)
    KcT = dscr("KcT", [256, S_LEN], BF16)
    Vc = dscr("Vc", [S_LEN, 4 * 64], BF16)
    MIXT = dscr("MIXT", [D, S_LEN], F32)
    dexp_s = dscr("dexp_s", [H_DIL, 128, 768], F32)
    mexp_s = dscr("mexp_s", [H_MOBA, 128, MSTRIP_W], F32)
    dbg = {}
    if debug:
        dbg['mix'] = nc.dram_tensor("dbg_mix", [D, S_LEN], F32, kind="ExternalOutput").ap()

    def gsb(name, shape, dt):
        return nc.alloc_sbuf_tensor("sb_" + name, list(shape), dt)

    gains = gsb("gains", [128, GPL * L + 8], F32)
    ones_bf = gsb("ones_bf", [128, 128], BF16)
    ident = gsb("ident", [128, 128], F32)
    sel65 = gsb("sel65", [128, 64], F32)
    tri = gsb("tri", [128, 128], BF16)
    mtab = gsb("mtab", [128, 768], F32)
    epst = gsb("epst", [128, 1], F32)
    kmT = gsb("kmT", [128, 2, 16], BF16)
    kmacc = gsb("kmacc", [128, 2, 16], F32)
    kmh_sb = gsb("kmh_sb", [64, 16], BF16)
    P = [nc.alloc_psum_tensor("P%d" % i, [128, 512], F32) for i in range(8)]
    PN = ["P%d" % i for i in range(8)]

    S.dma('sp', gains[:], gains_d, writes=['gains'])
    S.dma('sp', ident[:], ident_d, writes=['ident'])
    S.dma('sp', sel65[:], sel65_d, writes=['sel65'])
    S.dma('sp', mtab[:], mtab_d, writes=['mtab'])
    S.dma('pool', tri[:], tri_d, writes=['tri'])
    S.op('pool', lambda e: e.memset(ones_bf[:], 1.0), writes=['ones_bf'])
    S.op('pool', lambda e: e.memset(epst[:], EPS), writes=['epst'])

    def cast_weights(l, part):
        grp = ((win_f, win_b, 'win', 128, 8 * WIN_COLS), (wuq_f, wuq_b, 'wuq', 128, 3456),
               (wukv_f, wukv_b, 'wukv', 128, 768)) if part == 0 else ((wo_f, wo_b, 'wo', 128, 8192),)
        for src, dst, nm, rows, cols in grp:
            step = 4096
            for c0 in range(0, cols, step):
                c1 = min(cols, c0 + step)
                S.dma('pool', dst[l, :, c0:c1], src[l, :, c0:c1], writes=['%s_b%d_%d' % (nm, l, c0)])
        if part == 0:
            return
        for j in range(32):
            S.dma('pool', wup_b[l, j], wup_f[l, j], writes=['wup_b%d_%d' % (l, j)])
        for oc in range(8):
            S.dma('pool', wdn_b[l, oc], wdn_f[l, oc], writes=['wdn_b%d_%d' % (l, oc)])

    def wres(nm, l, cols, step=4096):
        return ['%s_b%d_%d' % (nm, l, c0) for c0 in range(0, cols, step)]

    cast_weights(0, 0)

    if True:
        tmp = gsb("su_tmp", [128, MSTRIP_W // 2], F32)
        for h in range(H_DIL):
            S.dma('sp', tmp[:, 0:768], dstrip_d[h], writes=['su_tmp'])
            S.op('act', lambda e: e.activation(out=tmp[:, 0:768], in_=tmp[:, 0:768], func=AF.Exp), reads=['su_tmp'], writes=['su_tmp'])
            S.dma('pool', dexp_s[h], tmp[:, 0:768], reads=['su_tmp'])
        for h in range(H_MOBA):
            for hf in range(2):
                cs_ = slice(hf * (MSTRIP_W // 2), (hf + 1) * (MSTRIP_W // 2))
                S.dma('sp', tmp[:], mstrip_d[h, :, cs_], writes=['su_tmp'])
                S.op('act', lambda e: e.activation(out=tmp[:], in_=tmp[:], func=AF.Exp), reads=['su_tmp'], writes=['su_tmp'])
                S.dma('pool', mexp_s[h, :, cs_], tmp[:], reads=['su_tmp'])

    def rstd_from(out_sb, in_ps, inv_n, rd, wr):
        npart = out_sb.shape[0]
        S.op('act', lambda e: e.activation(out=out_sb, in_=in_ps, func=AF.Ln, bias=epst[0:npart, :], scale=inv_n),
             reads=rd + ['epst'], writes=wr)
        S.op('act', lambda e: e.activation(out=out_sb, in_=out_sb, func=AF.Exp, scale=-0.5), reads=wr, writes=wr)

    def phase_P(l):
        gb = l * GPL
        src = xT_in if l == 0 else XT
        srcv = src.rearrange("(c p) s -> p c s", p=128)
        with ExitStack() as es:
            def sb(name, shape, dt):
                return es.enter_context(nc.sbuf_tensor(_uid("pp_" + name), list(shape), dt))
            win = sb("win", [128, 8, WIN_COLS], BF16)
            wuq = sb("wuq", [128, 3, 1152], BF16)
            wukv = sb("wukv", [128, 768], BF16)
            xt = [sb("xt%d" % i, [128, 8, TW], F32) for i in range(2)]
            cst = [sb("cst%d" % i, [128, 2, TW], F32) for i in range(2)]
            sq = sb("sq", [128, 8, TW], BF16)
            xg = sb("xg", [128, 8, TW], BF16)
            rbc = sb("rbc", [128, TW], F32)
            rcol = sb("rcol", [128, 4], F32)
            sQa = [sb("sQa%d" % i, [128, 3, TW], BF16) for i in range(2)]
            sKa = [sb("sKa%d" % i, [128, 3, TW], BF16) for i in range(2)]
            sQc = [sb("sQc%d" % i, [128, 2, TW], BF16) for i in range(2)]
            sKc = [sb("sKc%d" % i, [128, 2, TW], BF16) for i in range(2)]
            sQb = [sb("sQb%d" % i, [96, 6, TW], BF16) for i in range(2)]
            sKb = [sb("sKb%d" % i, [128, 3, TW], BF16) for i in range(2)]
            sKr = [sb("sKr%d" % i, [96, TW], BF16) for i in range(2)]
            sVa = [sb("sVa%d" % i, [128, 4, 6, 64], BF16) for i in range(2)]
            sVb = [sb("sVb%d" % i, [128, 4, 6, 64], BF16) for i in range(2)]
            sVc = [sb("sVc%d" % i, [128, 4, 4, 64], BF16) for i in range(2)]
            cq = sb("cq", [128, 3, TW], F32)
            cqs = sb("cqs", [128, 3, TW], BF16)
            cqg = sb("cqg", [128, 3, TW], BF16)
            rqbc = sb("rqbc", [128, TW], F32)
            ckv = sb("ckv", [128, TW], F32)
            ckvs = sb("ckvs", [128, TW], BF16)
            ckvg = sb("ckvg", [128, TW], BF16)
            rkbc = sb("rkbc", [128, TW], F32)
            rkcol = sb("rkcol", [128, 4], F32)
            t1 = sb("t1", [128, TW], F32)
            t2 = sb("t2", [128, TW], F32)
            S.op('pool', lambda e: e.memset(kmacc[:], 0.0), writes=['kmacc'])
            winv = win_b[l].rearrange("p (c n) -> p c n", c=8)
            for c in range(8):
                S.dma('sp', win[:, c, :], winv[:, c, :], reads=wres('win', l, 8 * WIN_COLS), writes=['win%d' % c])
            S.dma('sp', wuq[:], wuq_b[l].rearrange("p (c n) -> p c n", c=3), reads=wres('wuq', l, 3456), writes=['wuq'])
            S.dma('sp', wukv[:], wukv_b[l], reads=wres('wukv', l, 768), writes=['wukv'])
            frot = Rot([2, 3, 4])
            r = slice(64, 96)

            def load_x(t):
                ts_ = slice(t * TW, (t + 1) * TW)
                S.dma('sp', xt[t % 2][:], srcv[:, :, ts_], writes=['xt%d' % (t % 2)])
                S.dma('sp', cst[t % 2][64:96, :, :], rope_d[:, :, ts_].rearrange("a p s -> p a s"), writes=['cst%d' % (t % 2)])

            load_x(0)
            for t in range(NT):
                ts = slice(t * TW, (t + 1) * TW)
                tb = t % 2
                x_, xn = xt[tb], 'xt%d' % tb
                cs_, cn = cst[tb], 'cst%d' % tb
                if t + 1 < NT:
                    load_x(t + 1)
                S.op('act', lambda e: e.activation(out=sq[:], in_=x_[:], func=AF.Square), reads=[xn], writes=['sq'])
                for c in range(8):
                    S.op('dve', lambda e: e.tensor_scalar(out=xg[:, c, :], in0=x_[:, c, :], scalar1=gains[:, gb + c:gb + c + 1],
                                                          scalar2=None, op0=ALU.mult), reads=[xn, 'gains'], writes=['xg'])
                for c in range(8):
                    S.op('pe', lambda e: e.matmul(P[0][:], lhsT=ones_bf[:], rhs=sq[:, c, :], start=(c == 0), stop=(c == 7)),
                         reads=['ones_bf', 'sq'], writes=['P0'])
                rstd_from(rbc[:], P[0][:], 1.0 / D, ['P0'], ['rbc'])
                for j in range(4):
                    for c in range(8):
                        S.op('pe', lambda e: e.matmul(P[1][:, j:j + 1], lhsT=sq[:, c, j * 128:(j + 1) * 128], rhs=ones_bf[:, 0:1],
                                                      start=(c == 0), stop=(c == 7)), reads=['ones_bf', 'sq'], writes=['P1'])
                rstd_from(rcol[:], P[1][:, 0:4], 1.0 / D, ['P1'], ['rcol'])

                def fm_chunk(j):
                    b = frot.next()
                    for c in range(8):
                        S.op('pe', lambda e: e.matmul(P[b][:], lhsT=win[:, c, j * 128:(j + 1) * 128], rhs=xg[:, c, :],
                                                      start=(c == 0), stop=(c == 7)), reads=['win%d' % c, 'xg'], writes=[PN[b]])
                    return b

                def evac_bc(out_ap, b, bc, bcn, wr, np_=slice(0, 128)):
                    S.op('dve', lambda e: e.tensor_tensor(out=out_ap, in0=P[b][np_, :], in1=bc[np_, :], op=ALU.mult),
                         reads=[PN[b], bcn], writes=wr)

                def store_fm(dram, stage, sname):
                    S.dma('pool', dram.rearrange("(c p) s -> p c s", p=128)[:, :, ts], stage[:], reads=[sname])

                for j in range(16):
                    if j in (10, 11):
                        continue
                    b = fm_chunk(j)
                    if j < 3:
                        evac_bc(sQa[tb][:, j, :], b, rbc, 'rbc', ['sQa%d' % tb])
                        if j == 2:
                            store_fm(QaT, sQa[tb], 'sQa%d' % tb)
                    elif j < 6:
                        evac_bc(sKa[tb][:, j - 3, :], b, rbc, 'rbc', ['sKa%d' % tb])
                        if j == 5:
                            store_fm(KaT, sKa[tb], 'sKa%d' % tb)
                    elif j < 9:
                        evac_bc(cq[:, j - 6, :], b, rbc, 'rbc', ['cq'])
                    elif j == 9:
                        evac_bc(ckv[:], b, rbc, 'rbc', ['ckv'])
                    elif j < 14:
                        evac_bc(sQc[tb][:, j - 12, :], b, rbc, 'rbc', ['sQc%d' % tb])
                        if j == 13:
                            store_fm(QcT, sQc[tb], 'sQc%d' % tb)
                    else:
                        evac_bc(sKc[tb][:, j - 14, :], b, rbc, 'rbc', ['sKc%d' % tb])
                        S.op('dve', lambda e: e.tensor_reduce(out=kmacc[:, j - 14, 2 * t:2 * t + 2],
                                                              in_=sKc[tb][:, j - 14, :].rearrange("p (b k) -> p b k", k=256),
                                                              axis=AX.X, op=ALU.add), reads=['sKc%d' % tb], writes=['kmacc'])
                        if j == 15:
                            store_fm(KcT, sKc[tb], 'sKc%d' % tb)
                b10 = fm_chunk(10)
                b11 = fm_chunk(11)
                S.op('dve', lambda e: e.tensor_tensor(out=t1[r, :], in0=P[b10][r, :], in1=cs_[r, 0, :], op=ALU.mult),
                     reads=[PN[b10], cn], writes=['t1'])
                S.op('dve', lambda e: e.tensor_tensor(out=t2[r, :], in0=P[b11][r, :], in1=cs_[r, 1, :], op=ALU.mult),
                     reads=[PN[b11], cn], writes=['t2'])
                S.op('dve', lambda e: e.tensor_tensor(out=t1[r, :], in0=t1[r, :], in1=t2[r, :], op=ALU.add),
                     reads=['t1', 't2'], writes=['t1'])
                S.op('dve', lambda e: e.tensor_tensor(out=sKr[tb][r, :], in0=t1[r, :], in1=rbc[r, :], op=ALU.mult),
                     reads=['t1', 'rbc'], writes=['sKr%d' % tb])
                for h in range(6):
                    S.dma('pool', KbT[h, 64:96, ts], sKr[tb][r, :], reads=['sKr%d' % tb])
                for j in range(4):
                    js = slice(j * 128, (j + 1) * 128)
                    for c in range(8):
                        S.op('pe', lambda e: e.matmul(P[5][:, 0:384], lhsT=xg[:, c, js], rhs=win[:, c, 2048:2432],
                                                      start=(c == 0), stop=(c == 7)), reads=['xg', 'win%d' % c], writes=['P5'])
                    for c in range(8):
                        S.op('pe', lambda e: e.matmul(P[6][:, 0:256], lhsT=xg[:, c, js], rhs=win[:, c, 2432:2688],
                                                      start=(c == 0), stop=(c == 7)), reads=['xg', 'win%d' % c], writes=['P6'])
                    S.op('act', lambda e: e.activation(out=sVa[tb][:, j, :, :], in_=P[5][:, 0:384].rearrange("p (h e) -> p h e", e=64),
                                                       func=AF.Copy, scale=rcol[:, j:j + 1]),
                         reads=['P5', 'rcol'], writes=['sVa%d' % tb])
                    S.op('act', lambda e: e.activation(out=sVc[tb][:, j, :, :], in_=P[6][:, 0:256].rearrange("p (h e) -> p h e", e=64),
                                                       func=AF.Copy, scale=rcol[:, j:j + 1]),
                         reads=['P6', 'rcol'], writes=['sVc%d' % tb])
                S.dma('pool', Va.rearrange("(t j p) c -> t p j c", j=4, p=128)[t], sVa[tb][:].rearrange("p j h e -> p j (h e)"), reads=['sVa%d' % tb])
                S.dma('pool', Vc.rearrange("(t j p) c -> t p j c", j=4, p=128)[t], sVc[tb][:].rearrange("p j h e -> p j (h e)"), reads=['sVc%d' % tb])
                S.op('act', lambda e: e.activation(out=cqs[:], in_=cq[:], func=AF.Square), reads=['cq'], writes=['cqs'])
                for c in range(3):
                    S.op('pe', lambda e: e.matmul(P[0][:], lhsT=ones_bf[:], rhs=cqs[:, c, :], start=(c == 0), stop=(c == 2)),
                         reads=['ones_bf', 'cqs'], writes=['P0'])
                rstd_from(rqbc[:], P[0][:], 1.0 / 384, ['P0'], ['rqbc'])
                for c in range(3):
                    S.op('dve', lambda e: e.tensor_scalar(out=cqg[:, c, :], in0=cq[:, c, :], scalar1=gains[:, gb + 24 + c:gb + 25 + c],
                                                          scalar2=None, op0=ALU.mult), reads=['cq', 'gains'], writes=['cqg'])
                for h in range(6):
                    b1 = frot.next()
                    for c in range(3):
                        S.op('pe', lambda e: e.matmul(P[b1][0:96, :], lhsT=wuq[:, c, h * 192:h * 192 + 96], rhs=cqg[:, c, :],
                                                      start=(c == 0), stop=(c == 2)), reads=['wuq', 'cqg'], writes=[PN[b1]])
                    for c in range(3):
                        S.op('pe', lambda e: e.matmul(P[7][0:96, :], lhsT=wuq[:, c, h * 192 + 96:h * 192 + 192], rhs=cqg[:, c, :],
                                                      start=(c == 0), stop=(c == 2)), reads=['wuq', 'cqg'], writes=['P7'])
                    S.op('dve', lambda e: e.tensor_tensor(out=sQb[tb][0:64, h, :], in0=P[b1][0:64, :], in1=rqbc[0:64, :], op=ALU.mult),
                         reads=[PN[b1], 'rqbc'], writes=['sQb%d' % tb])
                    S.op('dve', lambda e: e.tensor_tensor(out=t1[r, :], in0=P[b1][r, :], in1=cs_[r, 0, :], op=ALU.mult),
                         reads=[PN[b1], cn], writes=['t1'])
                    S.op('dve', lambda e: e.tensor_tensor(out=t2[r, :], in0=P[7][r, :], in1=cs_[r, 1, :], op=ALU.mult),
                         reads=['P7', cn], writes=['t2'])
                    S.op('dve', lambda e: e.tensor_tensor(out=t1[r, :], in0=t1[r, :], in1=t2[r, :], op=ALU.add),
                         reads=['t1', 't2'], writes=['t1'])
                    S.op('dve', lambda e: e.tensor_tensor(out=sQb[tb][r, h, :], in0=t1[r, :], in1=rqbc[r, :], op=ALU.mult),
                         reads=['t1', 'rqbc'], writes=['sQb%d' % tb])
                S.dma('pool', QbT[:, :, ts].rearrange("h p s -> p h s"), sQb[tb][:], reads=['sQb%d' % tb])
                S.op('act', lambda e: e.activation(out=ckvs[:], in_=ckv[:], func=AF.Square), reads=['ckv'], writes=['ckvs'])
                S.op('pe', lambda e: e.matmul(P[0][:], lhsT=ones_bf[:], rhs=ckvs[:], start=True, stop=True),
                     reads=['ones_bf', 'ckvs'], writes=['P0'])
                rstd_from(rkbc[:], P[0][:], 1.0 / 128, ['P0'], ['rkbc'])
                for j in range(4):
                    S.op('pe', lambda e: e.matmul(P[1][:, j:j + 1], lhsT=ckvs[:, j * 128:(j + 1) * 128], rhs=ones_bf[:, 0:1],
                                                  start=True, stop=True), reads=['ones_bf', 'ckvs'], writes=['P1'])
                rstd_from(rkcol[:], P[1][:, 0:4], 1.0 / 128, ['P1'], ['rkcol'])
                S.op('dve', lambda e: e.tensor_scalar(out=ckvg[:], in0=ckv[:], scalar1=gains[:, gb + 27:gb + 28], scalar2=None,
                                                      op0=ALU.mult), reads=['ckv', 'gains'], writes=['ckvg'])
                for hp in range(3):
                    b = frot.next()
                    S.op('pe', lambda e: e.matmul(P[b][:], lhsT=wukv[:, hp * 128:(hp + 1) * 128], rhs=ckvg[:], start=True, stop=True),
                         reads=['wukv', 'ckvg'], writes=[PN[b]])
                    evac_bc(sKb[tb][:, hp, :], b, rkbc, 'rkbc', ['sKb%d' % tb])
                kbv = KbT[:, 0:64, ts].rearrange("(hp two) p s -> two p hp s", two=2)
                S.dma('pool', kbv[0], sKb[tb][0:64, :, :], reads=['sKb%d' % tb])
                S.dma('pool', kbv[1], sKb[tb][64:128, :, :], reads=['sKb%d' % tb])
                for j in range(4):
                    js = slice(j * 128, (j + 1) * 128)
                    S.op('pe', lambda e: e.matmul(P[5][:, 0:384], lhsT=ckvg[:, js], rhs=wukv[:, 384:768], start=True, stop=True),
                         reads=['ckvg', 'wukv'], writes=['P5'])
                    S.op('act', lambda e: e.activation(out=sVb[tb][:, j, :, :], in_=P[5][:, 0:384].rearrange("p (h e) -> p h e", e=64),
                                                       func=AF.Copy, scale=rkcol[:, j:j + 1]),
                         reads=['P5', 'rkcol'], writes=['sVb%d' % tb])
                S.dma('pool', Vb.rearrange("(t j p) c -> t p j c", j=4, p=128)[t], sVb[tb][:].rearrange("p j h e -> p j (h e)"), reads=['sVb%d' % tb])
            S.op('dve', lambda e: e.tensor_scalar(out=kmT[:], in0=kmacc[:], scalar1=1.0 / 256, scalar2=None, op0=ALU.mult),
                 reads=['kmacc'], writes=['kmT'])
            S.barrier()

    LA = 2

    def normalize_store(num_ap, z_ap, rd, lz_, lzn, o_, on, dst):
        S.op('act', lambda e: e.activation(out=lz_, in_=z_ap, func=AF.Ln), reads=rd, writes=[lzn])
        S.op('act', lambda e: e.activation(out=lz_, in_=lz_, func=AF.Exp, scale=-1.0), reads=[lzn], writes=[lzn])
        S.op('dve', lambda e: e.tensor_tensor(out=o_, in0=num_ap, in1=lz_, op=ALU.mult), reads=rd + [lzn], writes=[on])
        S.dma('pool', dst, o_, reads=[on])

    def phase_A(l):
        with ExitStack() as es:
            def sb(name, shape, dt):
                return es.enter_context(nc.sbuf_tensor(_uid("pa_" + name), list(shape), dt))
            qd = [[sb("q%d_%d" % (p, i), [128, S_LEN], BF16) for p in range(3)] for i in range(2)]
            kd = [[sb("k%d_%d" % (p, i), [128, S_LEN], BF16) for p in range(3)] for i in range(2)]
            vv = [[sb("v%d_%d" % (p, i), [128, 32, 128], BF16) for p in range(3)] for i in range(2)]
            dex = [sb("dex%d" % i, [128, 3, 256], F32) for i in range(2)]
            oT = [sb("oT%d" % i, [128, S_LEN], F32) for i in range(2)]
            E = [sb("E%d" % i, [128, 256], BF16) for i in range(4)]
            Pm = [sb("Pm%d" % i, [128, 256], BF16) for i in range(4)]
            lz = [sb("lz%d" % i, [64, TW], F32) for i in range(2)]
            osg = [sb("osg%d" % i, [64, TW], F32) for i in range(2)]
            for i in range(2):
                S.op('pool', lambda e: e.memset(qd[i][0][64:128, :], 0.0), writes=['pa_q0_%d' % i])
                S.op('pool', lambda e: e.memset(kd[i][0][64:128, :], 0.0), writes=['pa_k0_%d' % i])
                for p in range(3):
                    S.op('pool', lambda e: e.memset(vv[i][p][:, :, 64:128], 1.0), writes=['pa_vones%d_%d' % (p, i)])
            tiles = []
            for p in range(3):
                dil = PATTERNS[p][1]
                nA = S_LEN // dil // 128
                for kt in range(32):
                    a = kt % nA
                    nq = 256 if a + 1 < nA else 128
                    tiles.append((p, dil, kt // nA, a, kt, nq))

            def loads(h):
                hb = h % 2
                S.dma('sp', qd[hb][0][0:64, :], QaT[h * 64:(h + 1) * 64, :], writes=['pa_q0_%d' % hb])
                S.dma('sp', kd[hb][0][0:64, :], KaT[h * 64:(h + 1) * 64, :], writes=['pa_k0_%d' % hb])
                S.dma('sp', dex[hb][:], dexp_s[h].rearrange("p (a u) -> p a u", a=3), writes=['pa_dex%d' % hb])
                S.dma('sp', vv[hb][0][:, :, 0:64], Va.rearrange("(kt p) (h e) -> p kt h e", p=128, e=64)[:, :, h, :], writes=['pa_v0_%d_0' % hb])
                for p in (1, 2):
                    dil = PATTERNS[p][1]
                    nA = S_LEN // dil // 128
                    vsrc = Va.rearrange("(a j r) (h e) -> j r a h e", j=128, r=dil, e=64)
                    for r in range(dil):
                        S.dma('sp', vv[hb][p][:, r * nA:(r + 1) * nA, 0:64], vsrc[:, r, :, h, :], writes=['pa_v%d_%d_%d' % (p, hb, r)])
                    S.op('act', lambda e: e.activation(out=qd[hb][p][:].rearrange("p (r m) -> p r m", r=dil),
                                                       in_=qd[hb][0][:].rearrange("p (m r) -> p r m", r=dil), func=AF.Copy),
                         reads=['pa_q0_%d' % hb], writes=['pa_q%d_%d' % (p, hb)])
                    S.op('act', lambda e: e.activation(out=kd[hb][p][:].rearrange("p (r m) -> p r m", r=dil),
                                                       in_=kd[hb][0][:].rearrange("p (m r) -> p r m", r=dil), func=AF.Copy),
                         reads=['pa_k0_%d' % hb], writes=['pa_k%d_%d' % (p, hb)])

            def finalize(h):
                hb = h % 2
                on = 'pa_oT%d' % hb
                for t in range(NT):
                    ts = slice(t * TW, (t + 1) * TW)
                    normalize_store(oT[hb][0:64, ts], oT[hb][64:128, ts], [on], lz[t % 2][:], 'pa_lz%d' % (t % 2),
                                    osg[t % 2][:], 'pa_osg%d' % (t % 2), MIXT[h * 64:(h + 1) * 64, ts])

            srot = Rot([0, 1, 2, 3])
            orot = Rot([4, 5, 6])
            loads(0)
            S.op('pool', lambda e: e.memset(oT[0][:], 0.0), writes=['pa_oT0'])
            for h in range(H_DIL):
                hb = h % 2

                def qk(i):
                    p, dil, r, a, kt, nq = tiles[i]
                    b = srot.next()
                    S.op('pe', lambda e: e.matmul(P[b][:, 0:nq], lhsT=kd[hb][p][:, kt * 128:(kt + 1) * 128],
                                                  rhs=qd[hb][p][:, kt * 128:kt * 128 + nq], start=True, stop=True),
                         reads=['pa_k%d_%d' % (p, hb), 'pa_q%d_%d' % (p, hb)], writes=[PN[b]])
                    ei = i % 4
                    S.op('act', lambda e: e.activation(out=E[ei][:, 0:nq], in_=P[b][:, 0:nq], func=AF.Exp, scale=0.125),
                         reads=[PN[b]], writes=['pa_E%d' % ei])
                    S.op('dve', lambda e: e.tensor_tensor(out=Pm[ei][:, 0:nq], in0=E[ei][:, 0:nq], in1=dex[hb][:, p, 0:nq], op=ALU.mult),
                         reads=['pa_E%d' % ei, 'pa_dex%d' % hb], writes=['pa_Pm%d' % ei])

                def pv(i):
                    p, dil, r, a, kt, nq = tiles[i]
                    ei = i % 4
                    b = orot.next()
                    S.op('pe', lambda e: e.matmul(P[b][:, 0:nq], lhsT=vv[hb][p][:, kt, :], rhs=Pm[ei][:, 0:nq], start=True, stop=True),
                         reads=['pa_v%d_%d_%d' % (p, hb, r), 'pa_vones%d_%d' % (p, hb), 'pa_Pm%d' % ei], writes=[PN[b]])
                    ov = oT[hb][:].rearrange("p (m r) -> p r m", r=dil)[:, r, a * 128:a * 128 + nq]
                    S.op('dve', lambda e: e.tensor_tensor(out=ov, in0=P[b][:, 0:nq], in1=ov, op=ALU.add),
                         reads=[PN[b], 'pa_oT%d' % hb], writes=['pa_oT%d' % hb])

                n = len(tiles)
                for i in range(n + LA):
                    if i < n:
                        qk(i)
                    if i >= LA:
                        pv(i - LA)
                    if i == 6 and h + 1 < H_DIL:
                        loads(h + 1)
                    if i == 10:
                        if h > 0:
                            finalize(h - 1)
                        if h + 1 < H_DIL:
                            S.op('pool', lambda e: e.memset(oT[1 - hb][:], 0.0), writes=['pa_oT%d' % (1 - hb)])
            finalize(H_DIL - 1)
            S.barrier()

    def causal_phase(l, kind):
        moba = kind == 'c'
        nh = H_MOBA if moba else H_MLA
        dq = 128 if moba else 96
        scale = 0.125 if moba else 96 ** -0.5
        base_row = 768 if moba else 384
        with ExitStack() as es:
            def sb(name, shape, dt):
                return es.enter_context(nc.sbuf_tensor(_uid("pc_" + name), list(shape), dt))
            qT = [sb("qT%d" % i, [dq, S_LEN], BF16) for i in range(2)]
            kT = [sb("kT%d" % i, [dq, S_LEN], BF16) for i in range(2)]
            v = [sb("v%d" % i, [128, 32, 128], BF16) for i in range(2)]
            E = [sb("E%d" % i, [128, TW], BF16) for i in range(4)]
            lz = [sb("lz%d" % i, [64, TW], F32) for i in range(2)]
            osg = [sb("osg%d" % i, [64, TW], F32) for i in range(2)]
            for i in range(2):
                S.op('pool', lambda e: e.memset(v[i][:, :, 64:128], 1.0), writes=['pc_vones%d' % i])
            if moba:
                mex = [sb("mex%d" % i, [128, MSTRIP_W], F32) for i in range(2)]
                Pm = [sb("Pm%d" % i, [128, TW], BF16) for i in range(4)]
                kmh = [sb("kmh%d" % i, [128, 16], BF16) for i in range(2)]
                gm = [sb("gm%d" % i, [128, 16], F32) for i in range(4)]
                top8 = [sb("top8%d" % i, [128, 8], F32) for i in range(4)]
                selq = [sb("selq%d" % i, [128, 16], F32) for i in range(4)]
                for i in range(2):
                    S.op('pool', lambda e: e.memset(qT[i][64:128, :], 0.0), writes=['pc_qN%d' % i])
                    S.op('pool', lambda e: e.memset(kT[i][64:128, :], 0.0), writes=['pc_kN%d' % i])
                    S.op('pool', lambda e: e.memset(kmh[i][64:128, :], 0.0), writes=['pc_kmh%d' % i])
                    S.dma('pool', kT[i][64:80, :], blk1h_d, writes=['pc_kN%d' % i])

            def loads(h):
                hb = h % 2
                if moba:
                    S.dma('sp', qT[hb][0:64, :], QcT[h * 64:(h + 1) * 64, :], writes=['pc_qT%d' % hb])
                    S.dma('sp', kT[hb][0:64, :], KcT[h * 64:(h + 1) * 64, :], writes=['pc_kT%d' % hb])
                    S.dma('sp', v[hb][:, :, 0:64], Vc.rearrange("(kt p) (h e) -> p kt h e", p=128, e=64)[:, :, h, :], writes=['pc_v%d' % hb])
                    S.dma('sp', mex[hb][:], mexp_s[h], writes=['pc_mex%d' % hb])
                    S.dma('sp', kmh[hb][0:64, :], kmT[(h % 2) * 64:(h % 2) * 64 + 64, h // 2, :], reads=['kmT'], writes=['pc_kmh%d' % hb])
                else:
                    S.dma('sp', qT[hb][:], QbT[h], writes=['pc_qT%d' % hb])
                    S.dma('sp', kT[hb][:], KbT[h], writes=['pc_kT%d' % hb])
                    S.dma('sp', v[hb][:, :, 0:64], Vb.rearrange("(kt p) (h e) -> p kt h e", p=128, e=64)[:, :, h, :], writes=['pc_v%d' % hb])

            def prep_gate(h, i):
                hb = h % 2
                j = i // 2
                ib = i % 4
                g_, t8, sq_ = gm[ib], top8[ib], selq[ib]
                gn, tn, sn = 'pc_gm%d' % ib, 'pc_top8%d' % ib, 'pc_selq%d' % ib
                S.op('pe', lambda e: e.matmul(P[6][:, ib * 16:ib * 16 + 16], lhsT=qT[hb][:, i * 128:(i + 1) * 128], rhs=kmh[hb][:],
                                              start=True, stop=True), reads=['pc_qT%d' % hb, 'pc_kmh%d' % hb], writes=['P6_%d' % ib])
                S.op('dve', lambda e: e.tensor_tensor(out=g_[:], in0=P[6][:, ib * 16:ib * 16 + 16], in1=mtab[:, j * 16:(j + 1) * 16], op=ALU.add),
                     reads=['P6_%d' % ib, 'mtab'], writes=[gn])
                S.op('dve', lambda e: e.max(out=t8[:], in_=g_[:]), reads=[gn], writes=[tn])
                S.op('dve', lambda e: e.tensor_scalar(out=sq_[:], in0=g_[:], scalar1=t8[:, 2:3], scalar2=None, op0=ALU.is_ge),
                     reads=[gn, tn], writes=[sn])
                S.op('dve', lambda e: e.tensor_tensor(out=sq_[:], in0=sq_[:], in1=mtab[:, 256 + j * 16:256 + (j + 1) * 16], op=ALU.mult),
                     reads=[sn, 'mtab'], writes=[sn])
                S.op('dve', lambda e: e.tensor_tensor(out=sq_[:], in0=sq_[:], in1=mtab[:, 512 + j * 16:512 + (j + 1) * 16], op=ALU.add),
                     reads=[sn, 'mtab'], writes=[sn])
                S.op('dve', lambda e: e.tensor_scalar(out=sq_[:], in0=sq_[:], scalar1=-1.0, scalar2=-NEG, op0=ALU.add, op1=ALU.mult),
                     reads=[sn], writes=[sn])

            def prep_fin(h, i):
                hb = h % 2
                ib = i % 4
                sn = 'pc_selq%d' % ib
                tb_ = i % 2
                S.op('pe', lambda e: e.transpose(out=P[6][0:16, 128 + tb_ * 128:256 + tb_ * 128], in_=selq[ib][:], identity=ident[:]),
                     reads=[sn, 'ident'], writes=['P6t_%d' % tb_])
                S.op('act', lambda e: e.activation(out=qT[hb][64:80, i * 128:(i + 1) * 128], in_=P[6][0:16, 128 + tb_ * 128:256 + tb_ * 128], func=AF.Copy),
                     reads=['P6t_%d' % tb_], writes=['pc_qN%d' % hb])

            tiles = []
            for qg in range(NT):
                nk = 4 * qg + 4
                for kt in range(nk):
                    c = max(0, kt - 4 * qg)
                    tiles.append((qg, kt, c, kt == 0, kt == nk - 1))
            srot = Rot([0, 1, 2, 3])
            orot = Rot([4, 5])
            ob = [None]
            loads(0)
            if moba:
                for i in range(32):
                    prep_gate(0, i)
                    prep_fin(0, i)
            for h in range(nh):
                hb = h % 2

                def qk(i):
                    qg, kt, c, first, last = tiles[i]
                    c0 = c * 128
                    nq = TW - c0
                    q0 = qg * TW + c0
                    b = srot.next()
                    rd = ['pc_kT%d' % hb, 'pc_qT%d' % hb] + (['pc_kN%d' % hb, 'pc_qN%d' % hb] if moba else [])
                    S.op('pe', lambda e: e.matmul(P[b][:, 0:nq], lhsT=kT[hb][:, kt * 128:(kt + 1) * 128], rhs=qT[hb][:, q0:q0 + nq],
                                                  start=True, stop=True), reads=rd, writes=[PN[b]])
                    ei = i % 4
                    S.op('act', lambda e: e.activation(out=E[ei][:, 0:nq], in_=P[b][:, 0:nq], func=AF.Exp, scale=scale),
                         reads=[PN[b]], writes=['pc_E%d' % ei])
                    if moba:
                        u0 = min(q0 - kt * 128 + 384, MSTRIP_CLAMP)
                        S.op('dve', lambda e: e.tensor_tensor(out=Pm[ei][:, 0:nq], in0=E[ei][:, 0:nq], in1=mex[hb][:, u0:u0 + nq], op=ALU.mult),
                             reads=['pc_E%d' % ei, 'pc_mex%d' % hb], writes=['pc_Pm%d' % ei])
                    elif kt >= 4 * qg:
                        S.op('dve', lambda e: e.tensor_tensor(out=E[ei][:, 0:128], in0=E[ei][:, 0:128], in1=tri[:], op=ALU.mult),
                             reads=['pc_E%d' % ei, 'tri'], writes=['pc_E%d' % ei])

                def pv(i):
                    qg, kt, c, first, last = tiles[i]
                    c0 = c * 128
                    nq = TW - c0
                    ei = i % 4
                    if first:
                        ob[0] = orot.next()
                    b = ob[0]
                    src_, sn = (Pm[ei], 'pc_Pm%d' % ei) if moba else (E[ei], 'pc_E%d' % ei)
                    S.op('pe', lambda e: e.matmul(P[b][:, c0:TW], lhsT=v[hb][:, kt, :], rhs=src_[:, 0:nq], start=first, stop=last),
                         reads=['pc_v%d' % hb, 'pc_vones%d' % hb, sn], writes=[PN[b]])
                    if last:
                        normalize_store(P[b][0:64, :], P[b][64:128, :], [PN[b]], lz[qg % 2][:], 'pc_lz%d' % (qg % 2),
                                        osg[qg % 2][:], 'pc_osg%d' % (qg % 2),
                                        MIXT[base_row + h * 64:base_row + (h + 1) * 64, qg * TW:(qg + 1) * TW])

                n = len(tiles)
                for i in range(n + LA):
                    if i < n:
                        qk(i)
                    if i >= LA:
                        pv(i - LA)
                    if i == 6 and h + 1 < nh:
                        loads(h + 1)
                    if moba and h + 1 < nh and i >= 12:
                        k_, ph = divmod(i - 12, 4)
                        if k_ < 32 and ph == 0:
                            prep_gate(h + 1, k_)
                        if k_ < 32 and ph == 2:
                            prep_fin(h + 1, k_)
            S.barrier()

    def phase_O(l, last_layer):
        gb = l * GPL
        src = xT_in if l == 0 else XT
        srcv = src.rearrange("(c p) s -> p c s", p=128)
        dstv = XT.rearrange("(c p) s -> p c s", p=128)
        mixv = MIXT.rearrange("(c p) s -> p c s", p=128)
        yv = yT.rearrange("(c p) s -> p c s", p=128)
        with ExitStack() as es:
            def sb(name, shape, dt):
                return es.enter_context(nc.sbuf_tensor(_uid("po_" + name), list(shape), dt))
            wo = sb("wo", [128, 8, 1024], BF16)
            wup = [sb("wup%d" % i, [128, 4, 8, 128], BF16) for i in range(2)]
            wdn = [sb("wdn%d" % i, [128, 32, 128], BF16) for i in range(2)]
            xt = sb("xt", [128, 8, TW], F32)
            mx = sb("mx", [128, 8, TW], F32)
            sq = sb("sq", [128, 8, TW], BF16)
            mn = sb("mn", [128, 8, TW], BF16)
            rg = [sb("rg%d" % i, [128, TW], F32) for i in range(3)]
            x1 = sb("x1", [128, 8, TW], F32)
            hg = sb("hg", [128, 8, TW], BF16)
            rbc = sb("rbc", [128, TW], F32)
            r2 = sb("r2", [128, TW], F32)
            rr = sb("rr", [128, TW], BF16)
            u = sb("u", [128, 32, TW], BF16)
            yo = sb("yo", [128, 8, TW], F32)
            S.dma('sp', wo[:], wo_b[l].rearrange("p (c n) -> p c n", c=8), reads=wres('wo', l, 8192), writes=['po_wo'])
            prot = Rot([1, 2, 3, 4, 5, 6, 7])
            groups = ((0, 3), (3, 6), (6, 8))
            for t in range(NT):
                ts = slice(t * TW, (t + 1) * TW)
                S.dma('sp', xt[:], srcv[:, :, ts], writes=['po_xt'])
                S.dma('sp', mx[:], mixv[:, :, ts], writes=['po_mx'])
                S.op('act', lambda e: e.activation(out=sq[:], in_=mx[:], func=AF.Square), reads=['po_mx'], writes=['po_sq'])
                for gi, (c0, c1) in enumerate(groups):
                    for c in range(c0, c1):
                        S.op('pe', lambda e: e.matmul(P[0][:], lhsT=ones_bf[:], rhs=sq[:, c, :], start=(c == c0), stop=(c == c1 - 1)),
                             reads=['ones_bf', 'po_sq'], writes=['P0'])
                    rstd_from(rg[gi][:], P[0][:], 1.0 / ((c1 - c0) * 128), ['P0'], ['po_rg%d' % gi])
                    for c in range(c0, c1):
                        S.op('dve', lambda e: e.scalar_tensor_tensor(out=mn[:, c, :], in0=mx[:, c, :], scalar=gains[:, gb + 16 + c:gb + 17 + c],
                                                                     in1=rg[gi][:], op0=ALU.mult, op1=ALU.mult),
                             reads=['po_mx', 'gains', 'po_rg%d' % gi], writes=['po_mn'])
                for oc in range(8):
                    b = prot.next()
                    for c in range(8):
                        S.op('pe', lambda e: e.matmul(P[b][:], lhsT=wo[:, c, oc * 128:(oc + 1) * 128], rhs=mn[:, c, :],
                                                      start=(c == 0), stop=(c == 7)), reads=['po_wo', 'po_mn'], writes=[PN[b]])
                    S.op('dve', lambda e: e.tensor_tensor(out=x1[:, oc, :], in0=P[b][:], in1=xt[:, oc, :], op=ALU.add),
                         reads=[PN[b], 'po_xt'], writes=['po_x1'])
                S.op('act', lambda e: e.activation(out=sq[:], in_=x1[:], func=AF.Square), reads=['po_x1'], writes=['po_sq'])
                for c in range(8):
                    S.op('pe', lambda e: e.matmul(P[0][:], lhsT=ones_bf[:], rhs=sq[:, c, :], start=(c == 0), stop=(c == 7)),
                         reads=['ones_bf', 'po_sq'], writes=['P0'])
                rstd_from(rbc[:], P[0][:], 1.0 / D, ['P0'], ['po_rbc'])
                S.op('pool', lambda e: e.tensor_tensor(out=r2[:], in0=rbc[:], in1=rbc[:], op=ALU.mult), reads=['po_rbc'], writes=['po_r2'])
                for c in range(8):
                    S.op('dve', lambda e: e.tensor_scalar(out=hg[:, c, :], in0=x1[:, c, :], scalar1=gains[:, gb + 8 + c:gb + 9 + c],
                                                          scalar2=None, op0=ALU.mult), reads=['po_x1', 'gains'], writes=['po_hg'])
                for jg in range(8):
                    wb_ = wup[jg % 2]
                    wn = 'po_wup%d' % (jg % 2)
                    S.dma('sp', wb_[:], wup_b[l, jg * 4:(jg + 1) * 4].rearrange("j p (c m) -> p j c m", c=8),
                          reads=['wup_b%d_%d' % (l, jg * 4 + q) for q in range(4)], writes=[wn])
                    for jj in range(4):
                        j = jg * 4 + jj
                        b = prot.next()
                        for c in range(8):
                            S.op('pe', lambda e: e.matmul(P[b][:], lhsT=wb_[:, jj, c, :], rhs=hg[:, c, :], start=(c == 0), stop=(c == 7)),
                                 reads=[wn, 'po_hg'], writes=[PN[b]])
                        S.op('act', lambda e: e.activation(out=rr[:], in_=P[b][:], func=AF.Relu), reads=[PN[b]], writes=['po_rr'])
                        S.op('dve', lambda e: e.tensor_tensor(out=u[:, j, :], in0=rr[:], in1=rr[:], op=ALU.mult),
                             reads=['po_rr'], writes=['po_u'])
                for oc in range(8):
                    wb_ = wdn[oc % 2]
                    wn = 'po_wdn%d' % (oc % 2)
                    S.dma('sp', wb_[:], wdn_b[l, oc].rearrange("p (k m) -> p k m", k=32), reads=['wdn_b%d_%d' % (l, oc)], writes=[wn])
                    b = prot.next()
                    for k in range(32):
                        S.op('pe', lambda e: e.matmul(P[b][:], lhsT=wb_[:, k, :], rhs=u[:, k, :], start=(k == 0), stop=(k == 31)),
                             reads=[wn, 'po_u'], writes=[PN[b]])
                    S.op('dve', lambda e: e.tensor_tensor(out=yo[:, oc, :], in0=P[b][:], in1=r2[:], op=ALU.mult),
                         reads=[PN[b], 'po_r2'], writes=['po_yo'])
                    S.op('pool', lambda e: e.tensor_tensor(out=yo[:, oc, :], in0=yo[:, oc, :], in1=x1[:, oc, :], op=ALU.add),
                         reads=['po_yo', 'po_x1'], writes=['po_yo'])
                if not last_layer:
                    S.dma('pool', dstv[:, :, ts], yo[:], reads=['po_yo'])
                elif not final_norm:
                    S.dma('pool', yv[:, :, ts], yo[:], reads=['po_yo'])
                else:
                    gf = GPL * L
                    S.op('act', lambda e: e.activation(out=sq[:], in_=yo[:], func=AF.Square), reads=['po_yo'], writes=['po_sq'])
                    for c in range(8):
                        S.op('pe', lambda e: e.matmul(P[0][:], lhsT=ones_bf[:], rhs=sq[:, c, :], start=(c == 0), stop=(c == 7)),
                             reads=['ones_bf', 'po_sq'], writes=['P0'])
                    rstd_from(rbc[:], P[0][:], 1.0 / D, ['P0'], ['po_rbc'])
                    for c in range(8):
                        S.op('dve', lambda e: e.scalar_tensor_tensor(out=x1[:, c, :], in0=yo[:, c, :], scalar=gains[:, gf + c:gf + c + 1],
                                                                     in1=rbc[:], op0=ALU.mult, op1=ALU.mult),
                             reads=['po_yo', 'gains', 'po_rbc'], writes=['po_x1'])
                    S.dma('pool', yv[:, :, ts], x1[:], reads=['po_x1'])
            S.barrier()

    for l in range(L):
        if 'P' in phases:
            phase_P(l)
        cast_weights(l, 1)
        if l + 1 < L:
            cast_weights(l + 1, 0)
        if 'A' in phases:
            phase_A(l)
        if 'B' in phases:
            causal_phase(l, 'b')
        if 'C' in phases:
            causal_phase(l, 'c')
        if debug and l == 0:
            S.dma('sp', dbg['mix'], MIXT)
            S.barrier()
        if 'O' in phases:
            phase_O(l, l == L - 1)
    S.barrier()
    return nc, S


_HOST_INPUT_NAMES = ("w_in", "w_uq", "w_ukv", "w_o", "w_up", "w_down", "gains",
                     "dstrip", "mstrip", "rope", "ident", "sel65", "blk1h", "tri", "mtab")


def kernel(x, g_attn, w_in, g_q_lora, g_kv_lora, w_uq, w_ukv, rel_bias, g_mix, w_o, g_mlp, w_up, w_down, g_final,
           _depth=DEPTH, _debug=False):
    inp = dict(g_attn=np.asarray(g_attn), w_in=np.asarray(w_in), g_q_lora=np.asarray(g_q_lora), g_kv_lora=np.asarray(g_kv_lora),
               w_uq=np.asarray(w_uq), w_ukv=np.asarray(w_ukv), g_mix=np.asarray(g_mix), w_o=np.asarray(w_o),
               g_mlp=np.asarray(g_mlp), w_up=np.asarray(w_up), w_down=np.asarray(w_down), g_final=np.asarray(g_final))
    if _depth != DEPTH:
        for k in list(inp):
            if k != 'g_final':
                inp[k] = inp[k][:_depth]
    x = np.asarray(x)
    rel_bias = np.asarray(rel_bias, dtype=np.float32)
    if LAUNCH_MODE == 'multi' and not _debug:
        consts = _host_consts(rel_bias)
        progs = {}
        cur = [np.ascontiguousarray(x[c].T) for c in range(NCORES)]
        for l in range(_depth):
            last = l == _depth - 1
            if last not in progs:
                _UID[0] = 0
                progs[last] = build_program(1, False, 'PABCO', final_norm=last)[0]
            li = {k: (v if k == 'g_final' else v[l:l + 1]) for k, v in inp.items()}
            shared = _host_weights(li)
            shared.update(consts)
            in_maps = []
            for c in range(NCORES):
                m = {k: shared[k] for k in _HOST_INPUT_NAMES}
                m["xT"] = cur[c]
                in_maps.append(m)
            res = run_bass_kernel_spmd(progs[last], in_maps, core_ids=list(range(NCORES)))
            cur = [np.ascontiguousarray(res.results[c]["yT"]) for c in range(NCORES)]
        return np.stack([np.ascontiguousarray(cur[c].T) for c in range(NCORES)], 0).astype(np.float32)
    shared = _host_weights(inp)
    shared.update(_host_consts(rel_bias))
    nc, _ = build_program(_depth, _debug)
    in_maps = []
    for c in range(NCORES):
        m = {k: shared[k] for k in _HOST_INPUT_NAMES}
        m["xT"] = np.ascontiguousarray(x[c].T)
        in_maps.append(m)
    res = run_bass_kernel_spmd(nc, in_maps, core_ids=list(range(NCORES)))
    out = np.stack([np.ascontiguousarray(res.results[c]["yT"].T) for c in range(NCORES)], 0).astype(np.float32)
    if _debug:
        return out, res
    return out
```
